# Optimizing a Trainium2 kernel written in Bass

```python
import math
import jax, jax.numpy as jnp
from jax import lax
import numpy as np

D_MODEL = 1024
BATCH = 16
SEQ = 2048
DEPTH = 1

CHUNK = 64
SB_BLOCK = 128
SB_HEADS = 8
SB_HEAD_DIM = 64
SB_WIDTH = SB_HEADS * SB_HEAD_DIM
RET_HEADS = 8
RET_QK_DIM = 64
RET_V_DIM = 128
RET_QK_WIDTH = RET_HEADS * RET_QK_DIM
RET_V_WIDTH = RET_HEADS * RET_V_DIM
IN_COLS = 3 * SB_WIDTH + 2 * RET_QK_WIDTH + 2 * RET_V_WIDTH
N_BRANCHES = 2
ROPE_BASE = 10000.0
N_GROUPS = 4
EXPERTS_PER_GROUP = 4
N_EXPERTS = N_GROUPS * EXPERTS_PER_GROUP
TOP_K_IN_GROUP = 2
D_FF_EXPERT = 512
EPS = 1e-6

kernel_name = "hybrid_stickbreak_retention_hmoe"


def rms_norm(x, g):
    xf = x.astype(jnp.float32)
    y = xf * lax.rsqrt(jnp.mean(xf * xf, axis=-1, keepdims=True) + EPS)
    return (y * g.astype(jnp.float32)).astype(x.dtype)


def to_heads(t, n_heads):
    b, s, _ = t.shape
    return t.reshape(b, s, n_heads, -1).transpose(0, 2, 1, 3)


def from_heads(t):
    b, h, s, d = t.shape
    return t.transpose(0, 2, 1, 3).reshape(b, s, h * d)


def rotary(x, positions):
    half = x.shape[-1] // 2
    inv_freq = ROPE_BASE ** (-jnp.arange(half, dtype=jnp.float32) / half)
    ang = positions.astype(jnp.float32)[:, None] * inv_freq[None, :]
    cos, sin = jnp.cos(ang), jnp.sin(ang)
    x1, x2 = x[..., :half], x[..., half:]
    return jnp.concatenate([x1 * cos - x2 * sin, x2 * cos + x1 * sin], axis=-1)


def stick_breaking_attention(q, k, v):
    s_len, d = q.shape[2], q.shape[3]
    scale = 1.0 / math.sqrt(d)
    outs = []
    for blk in range(s_len // SB_BLOCK):
        q0 = blk * SB_BLOCK
        k_end = q0 + SB_BLOCK
        qb = q[:, :, q0:k_end]
        kb = k[:, :, :k_end]
        vb = v[:, :, :k_end]
        z = jnp.einsum('bhqd,bhkd->bhqk', qb, kb) * scale
        t_idx = q0 + jnp.arange(SB_BLOCK)[:, None]
        s_idx = jnp.arange(k_end)[None, :]
        mask = s_idx < t_idx
        log_keep = jnp.where(mask, jax.nn.log_sigmoid(-z), 0.0)
        suffix = lax.cumsum(log_keep, axis=3, reverse=True) - log_keep
        weights = jnp.where(mask, jnp.exp(jax.nn.log_sigmoid(z) + suffix), 0.0)
        outs.append(jnp.einsum('bhqk,bhkd->bhqd', weights, vb))
    return jnp.concatenate(outs, axis=2)


def retention_chunkwise(q, k, v):
    b, h, s_len, dk = q.shape
    dv = v.shape[-1]
    nc = s_len // CHUNK
    log_gamma = jnp.log(1.0 - 2.0 ** (-5.0 - jnp.arange(h, dtype=jnp.float32)))
    idx = jnp.arange(CHUNK, dtype=jnp.float32)
    dist = jnp.abs(idx[:, None] - idx[None, :])
    d_intra = jnp.exp(log_gamma[:, None, None] * dist)
    qc = q.reshape(b, h, nc, CHUNK, dk)
    kc = k.reshape(b, h, nc, CHUNK, dk)
    vc = v.reshape(b, h, nc, CHUNK, dv)
    scores = jnp.einsum('bhncd,bhnmd->bhncm', qc, kc) * d_intra[None, :, None]
    intra = jnp.einsum('bhncm,bhnme->bhnce', scores, vc)
    k_decay = jnp.exp(log_gamma[:, None] * (CHUNK - 1 - idx)[None, :])
    kv = jnp.einsum('bhnmd,bhnme->bhnde', kc * k_decay[None, :, None, :, None], vc)
    chunk_decay = jnp.exp(log_gamma * CHUNK)[None, :, None, None]

    def step(state, kv_n):
        return state * chunk_decay + kv_n, state

    init = jnp.zeros((b, h, dk, dv), jnp.float32)
    _, prev = lax.scan(step, init, jnp.moveaxis(kv, 2, 0))
    prev = jnp.moveaxis(prev, 0, 2)
    q_decay = jnp.exp(log_gamma[:, None] * (idx + 1.0)[None, :])
    cross = jnp.einsum('bhncd,bhnde->bhnce', qc * q_decay[None, :, None, :, None], prev)
    return (intra + cross).reshape(b, h, s_len, dv)


def hierarchical_moe(xn, w_group_router, b_group_router, w_expert_router, b_expert_router,
                     w_exp_gate, w_exp_up, w_exp_down):
    b, s_len, d = xn.shape
    xt = xn.reshape(b * s_len, d)
    n_tok = xt.shape[0]
    group_logits = (xt @ w_group_router + b_group_router).astype(jnp.float32)
    group_probs = jax.nn.softmax(group_logits, axis=-1)
    p_group, g_sel = lax.top_k(group_probs, 1)
    expert_logits = (xt @ w_expert_router + b_expert_router).astype(jnp.float32)
    expert_logits = expert_logits.reshape(n_tok, N_GROUPS, EXPERTS_PER_GROUP)
    in_group = jnp.take_along_axis(expert_logits, g_sel[:, :, None], axis=1)[:, 0]
    top_vals, top_idx = lax.top_k(in_group, TOP_K_IN_GROUP)
    gate = jax.nn.softmax(top_vals, axis=-1) * p_group
    expert_id = g_sel * EXPERTS_PER_GROUP + top_idx
    combine = jnp.sum(jax.nn.one_hot(expert_id, N_EXPERTS, dtype=jnp.float32) * gate[..., None], axis=1)
    combine = combine.astype(xt.dtype)
    y = jnp.zeros_like(xt)
    for e in range(N_EXPERTS):
        hidden = jax.nn.silu(xt @ w_exp_gate[e]) * (xt @ w_exp_up[e])
        y = y + combine[:, e:e + 1] * (hidden @ w_exp_down[e])
    return y.reshape(b, s_len, d)


def setup_inputs(seed: int = 0) -> dict:
    key = jax.random.key(seed)
    ks = jax.random.split(key, 20)
    f32 = jnp.float32

    def w(k, shape, fan_in):
        return jax.random.normal(k, shape, f32) * (fan_in ** -0.5)

    def gain(k, shape):
        return 1.0 + 0.02 * jax.random.normal(k, shape, f32)

    L = DEPTH
    return {
        "x": jax.random.normal(ks[0], (BATCH, SEQ, D_MODEL), f32),
        "norm_mix_g": gain(ks[1], (L, D_MODEL)),
        "w_in": w(ks[2], (L, D_MODEL, IN_COLS), D_MODEL),
        "w_gate": w(ks[3], (L, D_MODEL, N_BRANCHES * D_MODEL), D_MODEL),
        "b_gate": 0.02 * jax.random.normal(ks[4], (L, N_BRANCHES * D_MODEL), f32),
        "w_sb_out": w(ks[5], (L, SB_WIDTH, D_MODEL), SB_WIDTH),
        "w_ret_out": w(ks[6], (L, RET_V_WIDTH, D_MODEL), RET_V_WIDTH),
        "ret_norm_g": gain(ks[7], (L, RET_V_WIDTH)),
        "w_out": w(ks[8], (L, D_MODEL, D_MODEL), D_MODEL),
        "norm_ffn_g": gain(ks[9], (L, D_MODEL)),
        "w_group_router": w(ks[10], (L, D_MODEL, N_GROUPS), D_MODEL),
        "b_group_router": 0.01 * jax.random.normal(ks[11], (L, N_GROUPS), f32),
        "w_expert_router": w(ks[12], (L, D_MODEL, N_EXPERTS), D_MODEL),
        "b_expert_router": 0.01 * jax.random.normal(ks[13], (L, N_EXPERTS), f32),
        "w_exp_gate": w(ks[14], (L, N_EXPERTS, D_MODEL, D_FF_EXPERT), D_MODEL),
        "w_exp_up": w(ks[15], (L, N_EXPERTS, D_MODEL, D_FF_EXPERT), D_MODEL),
        "w_exp_down": w(ks[16], (L, N_EXPERTS, D_FF_EXPERT, D_MODEL), D_FF_EXPERT),
        "norm_final_g": gain(ks[17], (D_MODEL,)),
    }


def reference(x, norm_mix_g, w_in, w_gate, b_gate, w_sb_out, w_ret_out, ret_norm_g, w_out,
              norm_ffn_g, w_group_router, b_group_router, w_expert_router, b_expert_router,
              w_exp_gate, w_exp_up, w_exp_down, norm_final_g):
    h = x
    s_len = x.shape[1]
    positions = jnp.arange(s_len, dtype=jnp.int32)
    split_sizes = [SB_WIDTH, SB_WIDTH, SB_WIDTH, RET_QK_WIDTH, RET_QK_WIDTH, RET_V_WIDTH, RET_V_WIDTH]
    split_at = [int(i) for i in np.cumsum(split_sizes)[:-1]]
    for layer in range(DEPTH):
        xn = rms_norm(h, norm_mix_g[layer])
        proj = xn @ w_in[layer]
        sb_q, sb_k, sb_v, r_q, r_k, r_v, r_g = jnp.split(proj, split_at, axis=-1)
        y_sb = stick_breaking_attention(to_heads(sb_q, SB_HEADS).astype(jnp.float32),
                                        to_heads(sb_k, SB_HEADS).astype(jnp.float32),
                                        to_heads(sb_v, SB_HEADS).astype(jnp.float32))
        y_sb = from_heads(y_sb).astype(x.dtype) @ w_sb_out[layer]
        rq = rotary(to_heads(r_q, RET_HEADS).astype(jnp.float32), positions)
        rk = rotary(to_heads(r_k, RET_HEADS).astype(jnp.float32), positions) * (RET_QK_DIM ** -0.5)
        rv = to_heads(r_v, RET_HEADS).astype(jnp.float32)
        y_ret = retention_chunkwise(rq, rk, rv)
        y_ret = y_ret * lax.rsqrt(jnp.mean(y_ret * y_ret, axis=-1, keepdims=True) + EPS)
        y_ret = from_heads(y_ret) * ret_norm_g[layer].astype(jnp.float32)
        y_ret = (jax.nn.silu(r_g.astype(jnp.float32)) * y_ret).astype(x.dtype) @ w_ret_out[layer]
        gates = jax.nn.sigmoid(xn @ w_gate[layer] + b_gate[layer])
        g_sb, g_ret = jnp.split(gates, N_BRANCHES, axis=-1)
        h = h + (g_sb * y_sb + g_ret * y_ret) @ w_out[layer]
        hn = rms_norm(h, norm_ffn_g[layer])
        h = h + hierarchical_moe(hn, w_group_router[layer], b_group_router[layer],
                                 w_expert_router[layer], b_expert_router[layer],
                                 w_exp_gate[layer], w_exp_up[layer], w_exp_down[layer])
    return rms_norm(h, norm_final_g)
```

```python
import math
from contextlib import ExitStack

import numpy as np
import concourse.bass as bass
import concourse.mybir as mybir
from concourse.bass_utils import run_bass_kernel_spmd

F32 = mybir.dt.float32
BF16 = mybir.dt.bfloat16
AF = mybir.ActivationFunctionType
ALU = mybir.AluOpType
AX = mybir.AxisListType

NCORES = 8
SEQ_PER_CORE = 2
T = 2048
NT = 16
D = 1024
KC = 8
EPS = 1e-6
NEXP = 16

ENGS = ["pe", "act", "dve", "pool", "sp"]


def _rng(ap):
    sz = mybir.dt.size(ap.dtype)
    pairs = ap.ap
    pstep = pairs[0][0]
    off = ap.offset % pstep if pstep > 0 else ap.offset
    lo = off
    hi = off
    for st, cnt in pairs[1:]:
        if st >= 0:
            hi += st * (cnt - 1)
        else:
            lo += st * (cnt - 1)
    name = ap.tensor.name
    lo_b, hi_b = lo * sz, (hi + 1) * sz
    if name.startswith("psp"):
        lo_b = lo_b // 2048 * 2048
        hi_b = (hi_b + 2047) // 2048 * 2048
    return name, lo_b, hi_b


class Sched:
    def __init__(self):
        self.ops = []
        self.eng_ops = {e: [] for e in ENGS}
        self.reg = {}
        self.known = {e: {} for e in ENGS}
        self.slot_cnt = {}
        self.slot_last = {}

    def _acc(self, ap, opid, eng, is_write, deps):
        name, lo, hi = _rng(ap)
        lst = self.reg.setdefault(name, [])
        psum = name.startswith("psp")
        keep = []
        for ent in lst:
            elo, ehi, eop, ew, eeng = ent
            if ehi <= lo or elo >= hi or eop == opid:
                keep.append(ent)
                continue
            if ew or is_write or (psum and eeng != eng):
                deps.add(eop)
            if is_write and elo >= lo and ehi <= hi:
                continue
            if (not is_write) and (not ew) and eeng == eng and elo == lo and ehi == hi:
                continue
            keep.append(ent)
        keep.append((lo, hi, opid, is_write, eng))
        self.reg[name] = keep

    def add(self, eng, fn, reads=(), writes=(), slot=None, extra_deps=()):
        opid = len(self.ops)
        deps = set(extra_deps)
        for ap in reads:
            self._acc(ap, opid, eng, False, deps)
        for ap in writes:
            self._acc(ap, opid, eng, True, deps)
        if slot is not None and slot in self.slot_last:
            deps.add(self.slot_last[slot])
        op = dict(id=opid, eng=eng, fn=fn, slot=slot, waits=[], signal=False)
        op["pos"] = len(self.eng_ops[eng]) + 1
        if slot is not None:
            c = self.slot_cnt.get(slot, 0) + 1
            self.slot_cnt[slot] = c
            op["cnt"] = c
            self.slot_last[slot] = opid
        known = self.known[eng]
        need = {}
        for d in deps:
            Dop = self.ops[d]
            if Dop["slot"] is not None:
                key = ("dma", Dop["slot"])
                val = Dop["cnt"]
            else:
                key = Dop["eng"]
                val = Dop["pos"]
                if key == "pe" and eng == "pe":
                    continue
            if known.get(key, 0) >= val:
                continue
            if key not in need or need[key][0] < val:
                need[key] = (val, d)
        copied = False
        for key, (val, d) in sorted(need.items(), key=lambda kv: -kv[1][1]):
            if known.get(key, 0) >= val:
                continue
            Dop = self.ops[d]
            op["waits"].append((key, val))
            if Dop["slot"] is None:
                Dop["signal"] = True
            if not copied:
                known = dict(known)
                copied = True
            for k2, v2 in Dop["clk"].items():
                if known.get(k2, 0) < v2:
                    known[k2] = v2
            if known.get(key, 0) < val:
                known[key] = val
        if copied:
            self.known[eng] = known
        op["clk"] = known
        self.ops.append(op)
        self.eng_ops[eng].append(op)
        return opid

    def finish(self):
        self.add("sp", None, extra_deps=list(self.slot_last.values()))

    def emit(self, nc, stack):
        sems = {e: stack.enter_context(nc.semaphore("sem_" + e)) for e in ENGS}
        slot_sems = {}
        for s in self.slot_cnt:
            slot_sems[s] = stack.enter_context(nc.semaphore("dq_" + str(s)))
        sigval = {e: {} for e in ENGS}
        for e in ENGS:
            c = 0
            for op in self.eng_ops[e]:
                if op["signal"]:
                    c += 1
                    sigval[e][op["pos"]] = c
        block = stack.enter_context(nc.Block())

        def run(e, eng):
            for op in self.eng_ops[e]:
                for key, val in op["waits"]:
                    if isinstance(key, tuple):
                        eng.wait_ge(slot_sems[key[1]], 16 * val)
                    else:
                        eng.wait_ge(sems[key], sigval[key][val])
                if op["fn"] is None:
                    continue
                ins = op["fn"](eng)
                if op["slot"] is not None:
                    ins.then_inc(slot_sems[op["slot"]], 16)
                if op["signal"]:
                    ins.then_inc(sems[e], 1)

        @block.tensor
        def _(eng):
            run("pe", eng)

        @block.scalar
        def _(eng):
            run("act", eng)

        @block.vector
        def _(eng):
            run("dve", eng)

        @block.gpsimd
        def _(eng):
            run("pool", eng)

        @block.sync
        def _(eng):
            run("sp", eng)


def _view(reg, off, dtype, *free):
    sz = mybir.dt.size(dtype)
    n = 1
    for f in free:
        n *= f
    nbytes = n * sz
    assert off % 4 == 0 and nbytes % 4 == 0, (off, nbytes)
    ap = reg[:, off // 4:(off + nbytes) // 4]
    if dtype != F32:
        ap = ap.bitcast(dtype)
    if len(free) == 2:
        ap = ap.rearrange("p (a b) -> p a b", a=free[0], b=free[1])
    elif len(free) == 3:
        ap = ap.rearrange("p (a b c) -> p a b c", a=free[0], b=free[1], c=free[2])
    return ap


class _Stop(Exception):
    pass


class Carver:
    def __init__(self, reg, nbytes):
        self.reg = reg
        self.nbytes = nbytes
        self.off = 0

    def reset(self, off=0):
        self.off = off

    def get(self, dtype, *free):
        sz = mybir.dt.size(dtype)
        n = 1
        for f in free:
            n *= f
        nb = (n * sz + 3) // 4 * 4
        v = _view(self.reg, self.off, dtype, *free)
        self.off += nb
        assert self.off <= self.nbytes, (self.off, self.nbytes)
        return v


NWARM = 0


def build_program(stop=None, nseq=SEQ_PER_CORE):
    nc = bass.Bass("TRN2", target_bir_lowering=False)
    S = Sched()

    def din(name, shape):
        return nc.dram_tensor(name, list(shape), F32, kind="ExternalInput").ap()

    x_d = din("x", [SEQ_PER_CORE, T, D])
    g_mix_d = din("norm_mix_g", [D])
    w_in_d = din("w_in", [D, 4608])
    w_gate_d = din("w_gate", [D, 2048])
    bgate_d = din("b_gate_l", [128, 16])
    w_sbo_d = din("w_sb_out", [512, D])
    w_ro_d = din("w_ret_out", [D, D])
    rng_d = din("ret_norm_g_l", [128, 8])
    w_out_d = din("w_out", [D, D])
    g_ffn_d = din("norm_ffn_g", [D])
    w_gr_d = din("w_group_router", [D, 4])
    b_gr_d = din("b_group_router", [4])
    w_er_d = din("w_expert_router", [D, 16])
    b_er_d = din("b_expert_router", [16])
    w_eg_d = din("w_exp_gate", [NEXP, D, 512])
    w_eu_d = din("w_exp_up", [NEXP, D, 512])
    w_ed_d = din("w_exp_down", [NEXP, 512, D])
    g_fin_d = din("norm_final_g", [D])
    c_ident_d = din("c_ident", [128, 128])
    c_nti_d = din("c_nti", [128, 128])
    c_ntl_d = din("c_ntl", [128, 128])
    c_dmask_d = din("c_dmask", [128, 128])
    c_cos_d = din("c_cos", [128, 16, 32])
    c_sin_d = din("c_sin", [128, 16, 2, 32])
    c_decqk_d = din("c_decqk", [128, 16])
    c_mret_d = din("c_mret", [128, 8, 128])
    c_g128_d = din("c_g128", [128, 4])
    out_d = nc.dram_tensor("out", [SEQ_PER_CORE, T, D], F32, kind="ExternalOutput").ap()
    hscr_d = nc.dram_tensor("hscr", [SEQ_PER_CORE, T, D], F32).ap()
    dbg_d = nc.dram_tensor("dbg", [128, 16384], F32, kind="ExternalOutput").ap() if stop else None

    stack = ExitStack()
    with stack:
        def sb(name, nbytes):
            return stack.enter_context(nc.sbuf_tensor(name, [128, nbytes // 4], F32))

        R_act = sb("R_act", 32768)
        R_c = sb("R_c", 49152)
        R_m = sb("R_m", 32768)
        R_big = sb("R_big", 65536)
        R_misc = sb("R_misc", 28672)
        PSP = [stack.enter_context(nc.psum_tensor("psp%d" % i, [128, 1024], F32)) for i in range(4)]

        def bank(i):
            return PSP[i // 2][:, (i % 2) * 512:(i % 2) * 512 + 512]

        def bank_bf(i):
            return bank(i).bitcast(BF16)

        actT = _view(R_act, 0, BF16, 8, T)
        ysbT = _view(R_c, 0, BF16, 4, T)
        uT = _view(R_c, 16384, BF16, 8, T)
        mT = _view(R_m, 0, BF16, 8, T)

        misc = Carver(R_misc, 28672)
        xt = [misc.get(F32, D) for _ in range(2)]
        xn_bf = [misc.get(BF16, D) for _ in range(2)]
        junk = misc.get(BF16, D)
        gb = misc.get(F32, D)
        ident = misc.get(BF16, 128)
        nti = misc.get(BF16, 128)
        ntl = misc.get(BF16, 128)
        zero_bf = misc.get(BF16, 128)
        dmask = misc.get(F32, 128)
        decqk = misc.get(F32, 16)
        g128 = misc.get(F32, 4)
        bgate = misc.get(F32, 16)
        rngt = misc.get(F32, 8)
        eps_t = misc.get(F32, 1)
        wr_bf = misc.get(BF16, 8, 20)
        rbias = misc.get(F32, 20)
        ssq = [misc.get(F32, 2) for _ in range(4)]
        std = [misc.get(F32, 2) for _ in range(4)]
        rstd = [misc.get(F32, 2) for _ in range(4)]
        lg = misc.get(F32, 16, 20)
        r_gmax = misc.get(F32, 16)
        r_gd = misc.get(F32, 16, 4)
        r_gsum = misc.get(F32, 16)
        r_pg = misc.get(F32, 16)
        r_ohg = misc.get(F32, 16, 4)
        r_tmp = misc.get(F32, 16, 16)
        r_ig = misc.get(F32, 16, 4)
        r_ig2 = misc.get(F32, 16, 4)
        r_m1 = misc.get(F32, 16)
        r_m2 = misc.get(F32, 16)
        r_oh1 = misc.get(F32, 16, 4)
        r_oh2 = misc.get(F32, 16, 4)
        r_d = misc.get(F32, 16)
        r_w1 = misc.get(F32, 16)
        r_w2 = misc.get(F32, 16)
        r_cig = misc.get(F32, 16, 4)
        comb = misc.get(F32, 16, 16)

        def mm(out, lhsT, rhs, start=True, stop=True, skip=False):
            S.add("pe", lambda e: e.matmul(out, lhsT=lhsT, rhs=rhs, start=start, stop=stop, skip_group_check=skip),
                  reads=[lhsT, rhs], writes=[out])

        def tr(out, in_):
            S.add("pe", lambda e: e.transpose(out, in_, ident), reads=[in_, ident], writes=[out])

        def act(out, in_, func, scale=1.0, bias=None, accum_out=None):
            reads = [in_]
            writes = [out]
            kw = {}
            if bias is not None:
                kw["bias"] = bias
                if not isinstance(bias, (int, float)):
                    reads.append(bias)
            if not isinstance(scale, (int, float)):
                reads.append(scale)
            if accum_out is not None:
                kw["accum_out"] = accum_out
                writes.append(accum_out)
            S.add("act", lambda e: e.activation(out=out, in_=in_, func=func, scale=scale, **kw),
                  reads=reads, writes=writes)

        def tt(out, in0, in1, op, eng="dve"):
            S.add(eng, lambda e: e.tensor_tensor(out=out, in0=in0, in1=in1, op=op),
                  reads=[in0, in1], writes=[out])

        def ts(out, in0, s1, op0, s2=None, op1=None, eng="dve"):
            reads = [in0]
            if not isinstance(s1, (int, float)):
                reads.append(s1)
            if s2 is not None and not isinstance(s2, (int, float)):
                reads.append(s2)
            if op1 is None:
                S.add(eng, lambda e: e.tensor_scalar(out=out, in0=in0, scalar1=s1, scalar2=None, op0=op0),
                      reads=reads, writes=[out])
            else:
                S.add(eng, lambda e: e.tensor_scalar(out=out, in0=in0, scalar1=s1, scalar2=s2, op0=op0, op1=op1),
                      reads=reads, writes=[out])

        def stt(out, in0, scalar, in1, op0, op1, eng="dve"):
            reads = [in0, in1]
            if not isinstance(scalar, (int, float)):
                reads.append(scalar)
            S.add(eng, lambda e: e.scalar_tensor_tensor(out=out, in0=in0, scalar=scalar, in1=in1, op0=op0, op1=op1),
                  reads=reads, writes=[out])

        def cp(out, in_, eng="dve"):
            S.add(eng, lambda e: e.tensor_copy(out=out, in_=in_), reads=[in_], writes=[out])

        def red(out, in_, op, eng="dve"):
            S.add(eng, lambda e: e.tensor_reduce(out=out, in_=in_, axis=AX.X, op=op), reads=[in_], writes=[out])

        def recip(out, in_):
            S.add("dve", lambda e: e.reciprocal(out=out, in_=in_), reads=[in_], writes=[out])

        def memset(ap, val, eng="dve"):
            S.add(eng, lambda e: e.memset(ap, val), writes=[ap])

        def dma_in(eng, out, in_, slot, extra=()):
            return S.add(eng, lambda e: e.dma_start(out=out, in_=in_), writes=[out], slot=slot, extra_deps=extra)

        def dma_out(eng, out, in_, slot):
            return S.add(eng, lambda e: e.dma_start(out=out, in_=in_), reads=[in_], slot=slot)

        def wview(dram2d, r0, nrows, c0, ncols):
            return dram2d[r0:r0 + nrows, c0:c0 + ncols].rearrange("(kc p) n -> p kc n", p=128)

        memset(zero_bf, 0.0, eng="pool")
        memset(eps_t, EPS, eng="pool")
        dma_in("pool", ident, c_ident_d, "c0")
        dma_in("pool", nti, c_nti_d, "c1")
        dma_in("pool", ntl, c_ntl_d, "c2")
        dma_in("sp", dmask, c_dmask_d, "c3")
        dma_in("sp", decqk, c_decqk_d, "c4")
        dma_in("sp", g128, c_g128_d, "c5")
        dma_in("sp", bgate, bgate_d, "c6")
        dma_in("sp", rngt, rng_d, "c7")
        dma_in("pool", wr_bf[:, :, 0:4], wview(w_gr_d, 0, D, 0, 4), "c8")
        dma_in("pool", wr_bf[:, :, 4:20], wview(w_er_d, 0, D, 0, 16), "c9")
        dma_in("sp", rbias[:, 0:4], b_gr_d.partition_broadcast(128), "c10")
        dma_in("sp", rbias[:, 4:20], b_er_d.partition_broadcast(128), "c11")

        def rmsnorm_stats(src, k, inv_n, nparts=1):
            act(junk[:, 0:src.shape[1]], src, AF.Square, accum_out=ssq[k][:, 0:1])
            act(std[k][:, 0:1], ssq[k][:, 0:1], AF.Sqrt, scale=inv_n, bias=eps_t[:, 0:1])
            recip(rstd[k][:, 0:1], std[k][:, 0:1])

        def dump(view, n):
            S.add("pool", lambda e: e.dma_start(out=dbg_d[:, 0:n], in_=view), reads=[view], slot="dbg")

        def ck(tag, view, n):
            if stop == tag:
                dump(view, n)
                raise _Stop()

        def seq_body(b):
            big = Carver(R_big, 65536)

            dma_in("sp", gb, g_mix_d.partition_broadcast(128), "gb")
            xt4 = [xt[0], xt[1], _view(R_m, 0, F32, D), _view(R_m, 4096, F32, D)]

            def p1_a(i):
                s = i % 4
                dma_in("sp", xt4[s], x_d[b, i * 128:(i + 1) * 128, :], "xt%d" % s)
                rmsnorm_stats(xt4[s], s, 1.0 / D)
                stt(xn_bf[i % 2], xt4[s], rstd[s][:, 0:1], gb, ALU.mult, ALU.mult)

            def p1_b(i):
                pb = bank_bf(i % 2)
                for kc in range(KC):
                    tr(pb[:, kc * 128:(kc + 1) * 128], xn_bf[i % 2][:, kc * 128:(kc + 1) * 128])
                act(actT[:, :, i * 128:(i + 1) * 128], pb.rearrange("p (a b) -> p a b", a=8), AF.Copy)

            for i in range(NT + 1):
                if i < NT:
                    p1_a(i)
                if i >= 1:
                    p1_b(i - 1)

            if stop == "P1":
                dump(_view(R_act, 0, BF16, 16384), 16384)
                return

            big.reset(0)
            sbw = [big.get(BF16, 8, 384) for _ in range(2)]
            qT_s = [big.get(BF16, T) for _ in range(2)]
            kT_s = [[big.get(BF16, T) for _ in range(2)] for _ in range(2)]
            vpad_s = [[big.get(BF16, 16, 128) for _ in range(2)] for _ in range(2)]
            NES = 4
            rm2 = Carver(R_m, 32768)
            rm2.reset(8192)
            e_s = [rm2.get(F32, 512) for _ in range(NES)]
            sp_s = [rm2.get(BF16, 512) for _ in range(NES)]
            xs_s = [rm2.get(F32, 512) for _ in range(2)]
            W_s = [rm2.get(BF16, 512) for _ in range(2)]

            def load_sbw(hp):
                sl = hp % 2
                for j in range(3):
                    dma_in("pool", sbw[sl][:, :, j * 128:(j + 1) * 128],
                           wview(w_in_d, 0, D, j * 512 + hp * 128, 128), "sbw%d_%d" % (sl, j))

            for ps_ in range(2):
                memset(vpad_s[ps_][0][:, :, 64:128], 0.0)
                memset(vpad_s[ps_][1][:, :, 0:64], 0.0)
                memset(kT_s[ps_][0][64:128, :], 0.0)
                memset(kT_s[ps_][1][0:64, :], 0.0)

            def proj_items(hp):
                w = sbw[hp % 2]
                qT, kT, vpad = qT_s[hp % 2], kT_s[hp % 2], vpad_s[hp % 2]
                items = []
                cnt = [0]
                for which, dst, scl in ((0, qT, 0.125), (1, kT, 1.0)):
                    for tc in range(4):
                        def it(which=which, dst=dst, scl=scl, tc=tc):
                            pbk = bank(cnt[0] % 2)
                            cnt[0] += 1
                            for kc in range(KC):
                                mm(pbk, w[:, kc, which * 128:(which + 1) * 128], actT[:, kc, tc * 512:(tc + 1) * 512],
                                   start=(kc == 0), stop=(kc == KC - 1))
                            if which == 0:
                                ts(dst[:, tc * 512:(tc + 1) * 512], pbk, scl, ALU.mult)
                            else:
                                ts(dst[0][0:64, tc * 512:(tc + 1) * 512], pbk[0:64, :], scl, ALU.mult)
                                ts(dst[1][64:128, tc * 512:(tc + 1) * 512], pbk[64:128, :], scl, ALU.mult)
                        items.append(it)
                for quad in range(4):
                    def it(quad=quad):
                        pbk = bank(cnt[0] % 2)
                        cnt[0] += 1
                        for j in range(4):
                            i = quad * 4 + j
                            for kc in range(KC):
                                mm(pbk[:, j * 128:(j + 1) * 128], actT[:, kc, i * 128:(i + 1) * 128], w[:, kc, 256:384],
                                   start=(kc == 0), stop=(kc == KC - 1))
                        pv = pbk.rearrange("p (a b) -> p a b", a=4)
                        cp(vpad[0][:, quad * 4:(quad + 1) * 4, 0:64], pv[:, :, 0:64])
                        cp(vpad[1][:, quad * 4:(quad + 1) * 4, 64:128], pv[:, :, 64:128])
                    items.append(it)
                return items

            load_sbw(0)
            load_sbw(1)
            for it_ in proj_items(0):
                it_()
            for hp in range(4):
                qT, kT, vpad = qT_s[hp % 2], kT_s[hp % 2], vpad_s[hp % 2]
                nxt = proj_items(hp + 1) if hp + 1 < 4 else []
                units = [(c, kb, hl) for c in range(4) for kb in range(4 * c + 3, -1, -1) for hl in range(2)]
                NU = len(units)

                def geom(u):
                    c, kb, hl = units[u]
                    lo = max(0, kb - 4 * c) * 128
                    return c, kb, hl, lo, 512 - lo

                def s_qk(u):
                    c, kb, hl, lo, n = geom(u)
                    zb = bank(2 + u % 2)
                    mm(zb[:, 0:n], kT[hl][:, kb * 128:(kb + 1) * 128], qT[:, c * 512 + lo:(c + 1) * 512])

                def s_act1(u):
                    c, kb, hl, lo, n = geom(u)
                    zb = bank(2 + u % 2)
                    ee = e_s[u % NES]
                    spp = sp_s[u % NES]
                    act(ee[:, 0:n], zb[:, 0:n], AF.Exp)
                    act(spp[:, 0:n], ee[:, 0:n], AF.Ln, bias=1.0)
                    if kb >= 4 * c:
                        tt(ee[:, 0:128], ee[:, 0:128], dmask, ALU.mult)
                        tt(spp[:, 0:128], spp[:, 0:128], dmask, ALU.mult)

                def s_nti(u):
                    c, kb, hl, lo, n = geom(u)
                    Sb = bank(4 + hl)
                    if kb == 4 * c + 3:
                        mm(Sb, zero_bf, qT[:, 0:512], start=True, stop=True)
                    mm(Sb[:, lo:512], nti, sp_s[u % NES][:, 0:n], start=False, stop=True, skip=True)

                def s_exps(u):
                    c, kb, hl, lo, n = geom(u)
                    act(xs_s[u % 2][:, 0:n], bank(4 + hl)[:, lo:512], AF.Exp)

                def warm(k):
                    for _ in range(k):
                        mm(bank(7), zero_bf, qT[:, 0:512], start=True, stop=True)

                def s_ntl(u):
                    c, kb, hl, lo, n = geom(u)
                    warm(NWARM)
                    mm(bank(4 + hl)[:, lo:512], ntl, sp_s[u % NES][:, 0:n], start=False, stop=True, skip=True)

                def s_w(u):
                    c, kb, hl, lo, n = geom(u)
                    tt(W_s[u % 2][:, 0:n], e_s[u % NES][:, 0:n], xs_s[u % 2][:, 0:n], ALU.mult)

                def s_pv(u):
                    c, kb, hl, lo, n = geom(u)
                    Yb = bank(6 + c % 2)
                    if kb == 4 * c + 3 and hl == 0:
                        mm(Yb, zero_bf, qT[:, 0:512], start=True, stop=False)
                    last = (kb == 0 and hl == 1)
                    mm(Yb[:, lo:512], vpad[hl][:, kb, :], W_s[u % 2][:, 0:n], start=False, stop=last)
                    if last:
                        act(ysbT[:, hp, c * 512:(c + 1) * 512], Yb, AF.Copy)

                s_qk(0)
                for i in range(NU + 2):
                    if 0 <= i - 1 < NU:
                        s_nti(i - 1)
                    if i + 1 < NU:
                        s_qk(i + 1)
                    if i < NU:
                        s_act1(i)
                    if 0 <= i - 1 < NU:
                        s_exps(i - 1)
                    if 0 <= i - 2 < NU:
                        s_ntl(i - 2)
                        s_w(i - 2)
                        s_pv(i - 2)
                    if nxt and i >= 4 and (i - 4) % 6 == 0 and (i - 4) // 6 < len(nxt):
                        nxt[(i - 4) // 6]()
                    if i == 12 and hp + 2 < 4:
                        load_sbw(hp + 2)

            if stop == "P2":
                dump(_view(R_c, 0, BF16, 8192), 8192)
                return

            big.reset(0)
            retw = [big.get(BF16, 8, 768) for _ in range(2)]
            cosT = big.get(F32, 16, 32)
            sinS = big.get(F32, 16, 64)
            mret = big.get(F32, 8, 128)
            NS1 = 4
            t1_s = [big.get(F32, 256) for _ in range(NS1)]
            t2_s = [big.get(F32, 256) for _ in range(NS1)]
            qkd_s = [big.get(BF16, 256) for _ in range(NS1)]
            v_r_s = [big.get(BF16, 256) for _ in range(NS1)]
            sg_s = [big.get(F32, 256) for _ in range(5)]
            qkT_s = [big.get(BF16, 256) for _ in range(NS1)]
            Pm_s = [big.get(BF16, 256) for _ in range(2)]
            u_s = [big.get(BF16, 256) for _ in range(2)]
            Ysb_s = [big.get(F32, 256) for _ in range(4)]
            state_f = big.get(F32, 256)
            state_bd = big.get(BF16, 256)
            mhalf = big.get(F32, 2)
            var_s = [big.get(F32, 2) for _ in range(4)]

            def load_retw(hp):
                sl = hp % 2
                dma_in("pool", retw[sl][:, :, 0:128], wview(w_in_d, 0, D, 1536 + hp * 128, 128), "retw%d_0" % sl)
                dma_in("pool", retw[sl][:, :, 128:256], wview(w_in_d, 0, D, 2048 + hp * 128, 128), "retw%d_1" % sl)
                dma_in("pool", retw[sl][:, :, 256:512], wview(w_in_d, 0, D, 2560 + hp * 256, 256), "retw%d_2" % sl)
                dma_in("pool", retw[sl][:, :, 512:768], wview(w_in_d, 0, D, 3584 + hp * 256, 256), "retw%d_3" % sl)

            dma_in("sp", cosT, c_cos_d, "cos")
            dma_in("sp", sinS, c_sin_d.rearrange("p a b c -> p a (b c)"), "sin")
            dma_in("sp", mret, c_mret_d, "mret")
            memset(mhalf, -0.5, eng="pool")
            load_retw(0)
            if True:
                def st1(g, part):
                    hp, n = divmod(g, NT)
                    w = retw[hp % 2]
                    if part == 0 and n == 0 and hp + 1 < 4:
                        load_retw(hp + 1)
                    k = g % NS1
                    t1, t2, qkd, v_r, sg, qkT = t1_s[k], t2_s[k], qkd_s[k], v_r_s[k], sg_s[g % 5], qkT_s[k]
                    A = bank(0)
                    B = bank(1)
                    if part == 0:
                        for kc in range(KC):
                            mm(A, actT[:, kc, n * 128:(n + 1) * 128], w[:, kc, 0:512], start=(kc == 0), stop=(kc == KC - 1))
                        for kc in range(KC):
                            mm(B[:, 0:256], actT[:, kc, n * 128:(n + 1) * 128], w[:, kc, 512:768],
                               start=(kc == 0), stop=(kc == KC - 1))
                        return
                    if part == 2:
                        Tb = bank_bf(2)
                        tr(Tb[:, 0:128], qkd[:, 0:128])
                        tr(Tb[:, 128:256], qkd[:, 128:256])
                        act(qkT, Tb[:, 0:256], AF.Copy)
                        return
                    A4 = A[:, 0:256].rearrange("p (g h i) -> p g h i", g=4, h=2, i=32)
                    t1_4 = t1.rearrange("p (g h i) -> p g h i", g=4, h=2, i=32)
                    t2_4 = t2.rearrange("p (g h i) -> p g h i", g=4, h=2, i=32)
                    cos_b = cosT[:, n, :].unsqueeze(1).unsqueeze(1).broadcast_to([128, 4, 2, 32])
                    tt(t1_4, A4, cos_b, ALU.mult)
                    for hf in range(2):
                        sin_b = sinS[:, n, hf * 32:(hf + 1) * 32].unsqueeze(1).broadcast_to([128, 4, 32])
                        tt(t2_4[:, :, hf, :], A4[:, :, 1 - hf, :], sin_b, ALU.mult)
                    cp(v_r, A[:, 256:512])
                    act(sg, B[:, 0:256], AF.Silu)
                    tt(t1, t1, t2, ALU.add)
                    dq = decqk[:, hp * 4:(hp + 1) * 4].unsqueeze(2).broadcast_to([128, 4, 64])
                    tt(qkd.rearrange("p (g j) -> p g j", g=4), t1.rearrange("p (g j) -> p g j", g=4), dq, ALU.mult)

                def st2(g):
                    hp, n = divmod(g, NT)
                    if n == 0:
                        memset(state_f, 0.0)
                        memset(state_bd, 0.0)
                    k = g % NS1
                    k2 = g % 2
                    qkd, v_r, qkT, Pm = qkd_s[k], v_r_s[k], qkT_s[k], Pm_s[k2]
                    qdT = qkT[:, 0:128]
                    kdT = qkT[:, 128:256]
                    Scs = [bank(3), bank(7)]
                    for hl in range(2):
                        r0 = hl * 64
                        mm(Scs[hl][:, 0:128], kdT[r0:r0 + 64, :], qdT[r0:r0 + 64, :])
                    KV = bank(5)
                    mm(KV[:, 0:256], qkd[:, 128:256], v_r)
                    Y = bank(4)
                    mm(Y[:, 0:256], qdT, state_bd, start=True, stop=False)
                    for hl in range(2):
                        tt(Pm[:, hl * 128:(hl + 1) * 128], Scs[hl][:, 0:128], mret[:, 2 * hp + hl, :], ALU.mult)
                    for hl in range(2):
                        mm(Y[:, hl * 128:(hl + 1) * 128], Pm[:, hl * 128:(hl + 1) * 128], v_r[:, hl * 128:(hl + 1) * 128],
                           start=False, stop=(hl == 1))
                    for hl in range(2):
                        pr = slice(hl * 64, hl * 64 + 64)
                        cr = slice(hl * 128, hl * 128 + 128)
                        stt(state_f[pr, cr], state_f[pr, cr], g128[pr, hp:hp + 1], KV[pr, cr], ALU.mult, ALU.add)
                        act(state_bd[pr, cr], state_f[pr, cr], AF.Copy)
                    k4 = g % 4
                    for hl in range(2):
                        act(junk[:, 0:128], Y[:, hl * 128:(hl + 1) * 128], AF.Square, accum_out=ssq[k4][:, hl:hl + 1])
                    act(Ysb_s[k4], Y[:, 0:256], AF.Copy)

                def st2b(g):
                    k4 = g % 4
                    ts(var_s[k4], ssq[k4], 1.0 / 128, ALU.mult, EPS, ALU.add)
                    tt(rstd[k4], var_s[k4], mhalf, ALU.pow, eng="pool")

                def st3(g):
                    hp, n = divmod(g, NT)
                    k = g % NS1
                    k2 = g % 2
                    sg, u_t = sg_s[g % 5], u_s[k2]
                    k4 = g % 4
                    Ys = Ysb_s[k4]
                    for hl in range(2):
                        cr = slice(hl * 128, hl * 128 + 128)
                        stt(u_t[:, cr], Ys[:, cr], rstd[k4][:, hl:hl + 1], sg[:, cr], ALU.mult, ALU.mult)
                    T2 = bank_bf(6)
                    for hl in range(2):
                        tr(T2[:, hl * 128:(hl + 1) * 128], u_t[:, hl * 128:(hl + 1) * 128])
                    for hl in range(2):
                        h = 2 * hp + hl
                        act(uT[:, h, n * 128:(n + 1) * 128], T2[:, hl * 128:(hl + 1) * 128], AF.Copy, scale=rngt[:, h:h + 1])

                NG = 4 * NT
                for it_n in range(NG + 4):
                    if 0 <= it_n - 3 < NG:
                        st2b(it_n - 3)
                    if 0 <= it_n - 4 < NG:
                        st3(it_n - 4)
                    if it_n < NG:
                        st1(it_n, 0)
                        st1(it_n, 1)
                    if 0 <= it_n - 1 < NG:
                        st2(it_n - 1)
                    if it_n < NG:
                        st1(it_n, 2)

            if stop == "P3":
                dump(_view(R_c, 16384, BF16, 16384), 16384)
                return

            big.reset(0)
            a4w = []
            for _ in range(2):
                a4w.append(dict(wso=big.get(BF16, 4, 128), wro=big.get(BF16, 8, 128),
                                wg1=big.get(BF16, 8, 128), wg2=big.get(BF16, 8, 128)))
            a4s = []
            for _ in range(2):
                a4s.append(dict(s1=big.get(F32, 512), s2=big.get(F32, 512), m1=big.get(F32, 512), t=big.get(F32, 512)))
            wout = big.get(BF16, 8, D)

            def load_a4w(fc):
                sl = fc % 2
                dma_in("pool", a4w[sl]["wso"], wview(w_sbo_d, 0, 512, fc * 128, 128), "a4w%d_0" % sl)
                dma_in("pool", a4w[sl]["wro"], wview(w_ro_d, 0, D, fc * 128, 128), "a4w%d_1" % sl)
                dma_in("pool", a4w[sl]["wg1"], wview(w_gate_d, 0, D, fc * 128, 128), "a4w%d_2" % sl)
                dma_in("pool", a4w[sl]["wg2"], wview(w_gate_d, 0, D, 1024 + fc * 128, 128), "a4w%d_3" % sl)

            load_a4w(0)
            dma_in("pool", wout, wview(w_out_d, 0, D, 0, D), "wout")
            it = 0
            for fc in range(8):
                if fc + 1 < 8:
                    load_a4w(fc + 1)
                w = a4w[fc % 2]
                for tc in range(4):
                    cols = slice(tc * 512, (tc + 1) * 512)
                    bb = 4 * (it % 2)
                    sc = a4s[it % 2]
                    it += 1
                    A, B, C, Dd = bank(bb), bank(bb + 1), bank(bb + 2), bank(bb + 3)
                    for kc in range(4):
                        mm(A, w["wso"][:, kc, :], ysbT[:, kc, cols], start=(kc == 0), stop=(kc == 3))
                    for kc in range(8):
                        mm(B, w["wro"][:, kc, :], uT[:, kc, cols], start=(kc == 0), stop=(kc == 7))
                    for kc in range(8):
                        mm(C, w["wg1"][:, kc, :], actT[:, kc, cols], start=(kc == 0), stop=(kc == 7))
                    for kc in range(8):
                        mm(Dd, w["wg2"][:, kc, :], actT[:, kc, cols], start=(kc == 0), stop=(kc == 7))
                    act(sc["s1"], C, AF.Sigmoid, bias=bgate[:, fc:fc + 1])
                    act(sc["s2"], Dd, AF.Sigmoid, bias=bgate[:, 8 + fc:9 + fc])
                    tt(sc["m1"], A, sc["s1"], ALU.mult)
                    tt(sc["t"], B, sc["s2"], ALU.mult)
                    tt(mT[:, fc, cols], sc["m1"], sc["t"], ALU.add)

            if stop == "P4":
                dump(_view(R_m, 0, BF16, 16384), 16384)
                return

            moew = []
            for sl in range(2):
                base = sl * 24576
                moew.append(dict(wg=_view(R_c, base, BF16, 8, 512), wu=_view(R_c, base + 8192, BF16, 8, 512),
                                 wd=_view(R_c, base + 16384, BF16, 4, D)))
            def load_moew(e):
                sl = e % 2
                dma_in("pool", moew[sl]["wg"], w_eg_d[e].rearrange("(kc p) n -> p kc n", p=128), "moew%d_0" % sl)
                dma_in("pool", moew[sl]["wu"], w_eu_d[e].rearrange("(kc p) n -> p kc n", p=128), "moew%d_1" % sl)
                dma_in("pool", moew[sl]["wd"], w_ed_d[e].rearrange("(kc p) n -> p kc n", p=128), "moew%d_2" % sl)

            load_moew(0)

            dma_in("sp", gb, g_ffn_d.partition_broadcast(128), "gb")
            xt5 = [xt[0], xt[1], big.get(F32, D), big.get(F32, D)]

            h_store = {}

            def p5_a(i):
                s = i % 4
                dma_in("sp", xt5[s], x_d[b, i * 128:(i + 1) * 128, :], "xt%d" % s)
                pp = PSP[i % 2]
                for half in range(2):
                    for kc in range(KC):
                        mm(pp[:, half * 512:(half + 1) * 512], mT[:, kc, i * 128:(i + 1) * 128],
                           wout[:, kc, half * 512:(half + 1) * 512], start=(kc == 0), stop=(kc == KC - 1))
                tt(xt5[s], xt5[s], pp[:, :], ALU.add)
                h_store[i] = dma_out("pool", hscr_d[b, i * 128:(i + 1) * 128, :], xt5[s], "xs%d" % s)
                rmsnorm_stats(xt5[s], s, 1.0 / D)
                stt(xn_bf[i % 2], xt5[s], rstd[s][:, 0:1], gb, ALU.mult, ALU.mult)

            def p5_b(i):
                pb = bank_bf(4 + i % 2)
                for kc in range(KC):
                    tr(pb[:, kc * 128:(kc + 1) * 128], xn_bf[i % 2][:, kc * 128:(kc + 1) * 128])
                act(actT[:, :, i * 128:(i + 1) * 128], pb.rearrange("p (a b) -> p a b", a=8), AF.Copy)

            for i in range(NT + 1):
                if i < NT:
                    p5_a(i)
                if i >= 1:
                    p5_b(i - 1)

            if stop == "P5":
                dump(_view(R_act, 0, BF16, 16384), 16384)
                return

            yacc = _view(R_big, 0, F32, NT, D)
            mcar = Carver(R_m, 32768)
            hT = [mcar.get(BF16, 4, 512) for _ in range(2)]
            sgm = [mcar.get(F32, 512) for _ in range(2)]

            RB = bank(0)
            for i in range(NT):
                for kc in range(KC):
                    mm(RB[:, i * 20:(i + 1) * 20], actT[:, kc, i * 128:(i + 1) * 128], wr_bf[:, kc, :],
                       start=(kc == 0), stop=(kc == KC - 1))
            tt(lg, RB[:, 0:320].rearrange("p (a b) -> p a b", a=16), rbias.unsqueeze(1).broadcast_to([128, 16, 20]), ALU.add)
            GL = lg[:, :, 0:4]
            EL = lg[:, :, 4:20].rearrange("p t (g e) -> p t g e", g=4)

            def b4(v):
                return v.unsqueeze(2).broadcast_to([128, 16, 4])

            red(r_gmax, GL, ALU.max)
            tt(r_gd, GL, b4(r_gmax), ALU.subtract)
            act(r_gd, r_gd, AF.Exp)
            red(r_gsum, r_gd, ALU.add)
            recip(r_pg, r_gsum)
            tt(r_ohg, GL, b4(r_gmax), ALU.is_equal)
            tmp4 = r_tmp.rearrange("p t (g e) -> p t g e", g=4)
            tt(tmp4, EL, r_ohg.unsqueeze(3).broadcast_to([128, 16, 4, 4]), ALU.mult)
            red(r_ig, r_tmp.rearrange("p t (g e) -> p t e g", g=4), ALU.add)
            red(r_m1, r_ig, ALU.max)
            tt(r_oh1, r_ig, b4(r_m1), ALU.is_equal)
            stt(r_ig2, r_oh1, -1.0e30, r_ig, ALU.mult, ALU.add)
            red(r_m2, r_ig2, ALU.max)
            tt(r_oh2, r_ig2, b4(r_m2), ALU.is_equal)
            tt(r_d, r_m2, r_m1, ALU.subtract)
            act(r_d, r_d, AF.Exp)
            ts(r_w1, r_d, 1.0, ALU.add)
            recip(r_w1, r_w1)
            tt(r_w2, r_d, r_w1, ALU.mult)
            tt(r_w1, r_w1, r_pg, ALU.mult)
            tt(r_w2, r_w2, r_pg, ALU.mult)
            tt(r_cig, r_oh1, b4(r_w1), ALU.mult)
            tt(r_oh2, r_oh2, b4(r_w2), ALU.mult)
            tt(r_cig, r_cig, r_oh2, ALU.add)
            tt(comb.rearrange("p t (g e) -> p t g e", g=4), r_ohg.unsqueeze(3).broadcast_to([128, 16, 4, 4]),
               r_cig.unsqueeze(2).broadcast_to([128, 16, 4, 4]), ALU.mult)

            gcount = 0
            for e in range(NEXP):
                if e + 1 < NEXP:
                    load_moew(e + 1)
                w = moew[e % 2]
                for tc in range(4):
                    cols = slice(tc * 512, (tc + 1) * 512)
                    hh = hT[tc % 2]
                    for fcb in range(4):
                        G = bank(gcount % 2)
                        U = bank(2 + gcount % 2)
                        sgt = sgm[gcount % 2]
                        gcount += 1
                        for kc in range(KC):
                            mm(G, w["wg"][:, kc, fcb * 128:(fcb + 1) * 128], actT[:, kc, cols],
                               start=(kc == 0), stop=(kc == KC - 1))
                        for kc in range(KC):
                            mm(U, w["wu"][:, kc, fcb * 128:(fcb + 1) * 128], actT[:, kc, cols],
                               start=(kc == 0), stop=(kc == KC - 1))
                        act(sgt, G, AF.Silu)
                        tt(hh[:, fcb, :], sgt, U, ALU.mult)
                    for j in range(4):
                        i = tc * 4 + j
                        yp = PSP[2 + i % 2]
                        for half in range(2):
                            for fcb in range(4):
                                mm(yp[:, half * 512:(half + 1) * 512], hh[:, fcb, j * 128:(j + 1) * 128],
                                   w["wd"][:, fcb, half * 512:(half + 1) * 512], start=(fcb == 0), stop=(fcb == 3))
                        if e == 0:
                            ts(yacc[:, i, :], yp[:, :], comb[:, i, e:e + 1], ALU.mult)
                        else:
                            stt(yacc[:, i, :], yp[:, :], comb[:, i, e:e + 1], yacc[:, i, :], ALU.mult, ALU.add)

            dma_in("sp", gb, g_fin_d.partition_broadcast(128), "gb")
            xt6 = [xt[0], xt[1], _view(R_m, 16384, F32, D), _view(R_m, 20480, F32, D)]

            def fin_load(i):
                dma_in("sp", xt6[i % 4], hscr_d[b, i * 128:(i + 1) * 128, :], "xt%d" % (i % 4), extra=[h_store[i]])

            for i in range(3):
                fin_load(i)
            for i in range(NT):
                s = i % 4
                tt(xt6[s], xt6[s], yacc[:, i, :], ALU.add)
                rmsnorm_stats(xt6[s], s, 1.0 / D)
                stt(xt6[s], xt6[s], rstd[s][:, 0:1], gb, ALU.mult, ALU.mult)
                if i + 3 < NT:
                    fin_load(i + 3)
                dma_out("pool", out_d[b, i * 128:(i + 1) * 128, :], xt6[s], "xs%d" % s)

        try:
            for b in range(nseq):
                seq_body(b)
        except _Stop:
            pass

        S.finish()
        S.emit(nc, stack)
    return nc


def _constants():
    c = {}
    c["c_ident"] = np.eye(128, dtype=np.float32)
    j = np.arange(128)[:, None]
    s = np.arange(128)[None, :]
    c["c_nti"] = np.where(j >= s, -1.0, 0.0).astype(np.float32)
    c["c_ntl"] = np.where(j < s, -1.0, 0.0).astype(np.float32)
    c["c_dmask"] = np.where(j < s, 1.0, 0.0).astype(np.float32)
    inv_freq = (np.float32(10000.0) ** (-(np.arange(32, dtype=np.float32) / np.float32(32)))).astype(np.float32)
    pos = np.arange(T, dtype=np.float32)
    ang = (pos[:, None] * inv_freq[None, :]).astype(np.float32).astype(np.float64)
    cos = np.cos(ang).reshape(16, 128, 32).transpose(1, 0, 2)
    sin = np.sin(ang).reshape(16, 128, 32).transpose(1, 0, 2)
    c["c_cos"] = np.ascontiguousarray(cos).astype(np.float32)
    c["c_sin"] = np.ascontiguousarray(np.stack([-sin, sin], axis=2)).astype(np.float32)
    log_gamma = np.log(1.0 - 2.0 ** (-5.0 - np.arange(8, dtype=np.float64)))
    p = np.arange(128, dtype=np.float64)
    decqk = np.zeros((128, 16), np.float64)
    for hp in range(4):
        for hl in range(2):
            h = 2 * hp + hl
            decqk[:, hp * 4 + hl] = np.exp(log_gamma[h] * (p + 1.0))
            decqk[:, hp * 4 + 2 + hl] = 0.125 * np.exp(log_gamma[h] * (127.0 - p))
    c["c_decqk"] = decqk.astype(np.float32)
    m = np.arange(128)[:, None].astype(np.float64)
    cc = np.arange(128)[None, :].astype(np.float64)
    valid = (np.arange(128)[:, None] // 64) <= (np.arange(128)[None, :] // 64)
    mret = np.zeros((128, 8, 128), np.float64)
    for h in range(8):
        mret[:, h, :] = np.where(valid, np.exp(log_gamma[h] * (np.abs(cc - m) - (cc - m) - 128.0)), 0.0)
    c["c_mret"] = mret.astype(np.float32)
    g128 = np.zeros((128, 4), np.float64)
    for hp in range(4):
        g128[0:64, hp] = np.exp(log_gamma[2 * hp] * 128.0)
        g128[64:128, hp] = np.exp(log_gamma[2 * hp + 1] * 128.0)
    c["c_g128"] = g128.astype(np.float32)
    return c


_NC_CACHE = {}


def kernel(x, norm_mix_g, w_in, w_gate, b_gate, w_sb_out, w_ret_out, ret_norm_g, w_out,
           norm_ffn_g, w_group_router, b_group_router, w_expert_router, b_expert_router,
           w_exp_gate, w_exp_up, w_exp_down, norm_final_g):
    f = lambda a: np.ascontiguousarray(np.asarray(a, dtype=np.float32))
    if "nc" not in _NC_CACHE:
        _NC_CACHE["nc"] = build_program()
    nc = _NC_CACHE["nc"]
    shared = {
        "norm_mix_g": f(norm_mix_g).reshape(D),
        "w_in": f(w_in).reshape(D, 4608),
        "w_gate": f(w_gate).reshape(D, 2048),
        "b_gate_l": f(f(b_gate).reshape(16, 128).T),
        "w_sb_out": f(w_sb_out).reshape(512, D),
        "w_ret_out": f(w_ret_out).reshape(D, D),
        "ret_norm_g_l": f(f(ret_norm_g).reshape(8, 128).T),
        "w_out": f(w_out).reshape(D, D),
        "norm_ffn_g": f(norm_ffn_g).reshape(D),
        "w_group_router": f(w_group_router).reshape(D, 4),
        "b_group_router": f(b_group_router).reshape(4),
        "w_expert_router": f(w_expert_router).reshape(D, 16),
        "b_expert_router": f(b_expert_router).reshape(16),
        "w_exp_gate": f(w_exp_gate).reshape(NEXP, D, 512),
        "w_exp_up": f(w_exp_up).reshape(NEXP, D, 512),
        "w_exp_down": f(w_exp_down).reshape(NEXP, 512, D),
        "norm_final_g": f(norm_final_g).reshape(D),
    }
    shared.update(_constants())
    xf = f(x)
    in_maps = []
    for c in range(NCORES):
        m = dict(shared)
        m["x"] = np.ascontiguousarray(xf[c * SEQ_PER_CORE:(c + 1) * SEQ_PER_CORE])
        in_maps.append(m)
    res = run_bass_kernel_spmd(nc, in_maps, core_ids=list(range(NCORES)))
    out = np.concatenate([np.asarray(r["out"], dtype=np.float32) for r in res.results], axis=0)
    return out.reshape(16, T, D)
```

```python
import math
from contextlib import ExitStack

import numpy as np
import concourse.bass as bass
import concourse.mybir as mybir
from concourse.bass_utils import run_bass_kernel_spmd

F32 = mybir.dt.float32
BF16 = mybir.dt.bfloat16
AF = mybir.ActivationFunctionType
ALU = mybir.AluOpType
AX = mybir.AxisListType

NCORES = 8
SEQ_PER_CORE = 2
T = 2048
NT = 16
D = 1024
KC = 8
EPS = 1e-6
NEXP = 16

ENGS = ["pe", "act", "dve", "pool", "sp"]


def _rng(ap):
    sz = mybir.dt.size(ap.dtype)
    pairs = ap.ap
    pstep = pairs[0][0]
    off = ap.offset % pstep if pstep > 0 else ap.offset
    lo = off
    hi = off
    for st, cnt in pairs[1:]:
        if st >= 0:
            hi += st * (cnt - 1)
        else:
            lo += st * (cnt - 1)
    name = ap.tensor.name
    lo_b, hi_b = lo * sz, (hi + 1) * sz
    if name.startswith("psp"):
        lo_b = lo_b // 2048 * 2048
        hi_b = (hi_b + 2047) // 2048 * 2048
    return name, lo_b, hi_b


class Sched:
    def __init__(self):
        self.ops = []
        self.eng_ops = {e: [] for e in ENGS}
        self.reg = {}
        self.known = {e: {} for e in ENGS}
        self.slot_cnt = {}
        self.slot_last = {}

    def _acc(self, ap, opid, eng, is_write, deps):
        name, lo, hi = _rng(ap)
        lst = self.reg.setdefault(name, [])
        psum = name.startswith("psp")
        keep = []
        for ent in lst:
            elo, ehi, eop, ew, eeng = ent
            if ehi <= lo or elo >= hi or eop == opid:
                keep.append(ent)
                continue
            if ew or is_write or (psum and eeng != eng):
                deps.add(eop)
            if is_write and elo >= lo and ehi <= hi:
                continue
            if (not is_write) and (not ew) and eeng == eng and elo == lo and ehi == hi:
                continue
            keep.append(ent)
        keep.append((lo, hi, opid, is_write, eng))
        self.reg[name] = keep

    def add(self, eng, fn, reads=(), writes=(), slot=None, extra_deps=()):
        opid = len(self.ops)
        deps = set(extra_deps)
        for ap in reads:
            self._acc(ap, opid, eng, False, deps)
        for ap in writes:
            self._acc(ap, opid, eng, True, deps)
        if slot is not None and slot in self.slot_last:
            deps.add(self.slot_last[slot])
        op = dict(id=opid, eng=eng, fn=fn, slot=slot, waits=[], signal=False)
        op["pos"] = len(self.eng_ops[eng]) + 1
        if slot is not None:
            c = self.slot_cnt.get(slot, 0) + 1
            self.slot_cnt[slot] = c
            op["cnt"] = c
            self.slot_last[slot] = opid
        known = self.known[eng]
        need = {}
        for d in deps:
            Dop = self.ops[d]
            if Dop["slot"] is not None:
                key = ("dma", Dop["slot"])
                val = Dop["cnt"]
            else:
                key = Dop["eng"]
                val = Dop["pos"]
                if key == "pe" and eng == "pe":
                    continue
            if known.get(key, 0) >= val:
                continue
            if key not in need or need[key][0] < val:
                need[key] = (val, d)
        copied = False
        for key, (val, d) in sorted(need.items(), key=lambda kv: -kv[1][1]):
            if known.get(key, 0) >= val:
                continue
            Dop = self.ops[d]
            op["waits"].append((key, val))
            if Dop["slot"] is None:
                Dop["signal"] = True
            if not copied:
                known = dict(known)
                copied = True
            for k2, v2 in Dop["clk"].items():
                if known.get(k2, 0) < v2:
                    known[k2] = v2
            if known.get(key, 0) < val:
                known[key] = val
        if copied:
            self.known[eng] = known
        op["clk"] = known
        self.ops.append(op)
        self.eng_ops[eng].append(op)
        return opid

    def finish(self):
        self.add("sp", None, extra_deps=list(self.slot_last.values()))

    def emit(self, nc, stack):
        sems = {e: stack.enter_context(nc.semaphore("sem_" + e)) for e in ENGS}
        slot_sems = {}
        for s in self.slot_cnt:
            slot_sems[s] = stack.enter_context(nc.semaphore("dq_" + str(s)))
        sigval = {e: {} for e in ENGS}
        for e in ENGS:
            c = 0
            for op in self.eng_ops[e]:
                if op["signal"]:
                    c += 1
                    sigval[e][op["pos"]] = c
        block = stack.enter_context(nc.Block())

        def run(e, eng):
            for op in self.eng_ops[e]:
                for key, val in op["waits"]:
                    if isinstance(key, tuple):
                        eng.wait_ge(slot_sems[key[1]], 16 * val)
                    else:
                        eng.wait_ge(sems[key], sigval[key][val])
                if op["fn"] is None:
                    continue
                ins = op["fn"](eng)
                if op["slot"] is not None:
                    ins.then_inc(slot_sems[op["slot"]], 16)
                if op["signal"]:
                    ins.then_inc(sems[e], 1)

        @block.tensor
        def _(eng):
            run("pe", eng)

        @block.scalar
        def _(eng):
            run("act", eng)

        @block.vector
        def _(eng):
            run("dve", eng)

        @block.gpsimd
        def _(eng):
            run("pool", eng)

        @block.sync
        def _(eng):
            run("sp", eng)


def _view(reg, off, dtype, *free):
    sz = mybir.dt.size(dtype)
    n = 1
    for f in free:
        n *= f
    nbytes = n * sz
    assert off % 4 == 0 and nbytes % 4 == 0, (off, nbytes)
    ap = reg[:, off // 4:(off + nbytes) // 4]
    if dtype != F32:
        ap = ap.bitcast(dtype)
    if len(free) == 2:
        ap = ap.rearrange("p (a b) -> p a b", a=free[0], b=free[1])
    elif len(free) == 3:
        ap = ap.rearrange("p (a b c) -> p a b c", a=free[0], b=free[1], c=free[2])
    return ap


class _Stop(Exception):
    pass


class Carver:
    def __init__(self, reg, nbytes):
        self.reg = reg
        self.nbytes = nbytes
        self.off = 0

    def reset(self, off=0):
        self.off = off

    def get(self, dtype, *free):
        sz = mybir.dt.size(dtype)
        n = 1
        for f in free:
            n *= f
        nb = (n * sz + 3) // 4 * 4
        v = _view(self.reg, self.off, dtype, *free)
        self.off += nb
        assert self.off <= self.nbytes, (self.off, self.nbytes)
        return v


NWARM = 0


def build_program(stop=None, nseq=SEQ_PER_CORE):
    nc = bass.Bass("TRN2", target_bir_lowering=False)
    S = Sched()

    def din(name, shape):
        return nc.dram_tensor(name, list(shape), F32, kind="ExternalInput").ap()

    x_d = din("x", [SEQ_PER_CORE, T, D])
    g_mix_d = din("norm_mix_g", [D])
    w_in_d = din("w_in", [D, 4608])
    w_gate_d = din("w_gate", [D, 2048])
    bgate_d = din("b_gate_l", [128, 16])
    w_sbo_d = din("w_sb_out", [512, D])
    w_ro_d = din("w_ret_out", [D, D])
    rng_d = din("ret_norm_g_l", [128, 8])
    w_out_d = din("w_out", [D, D])
    g_ffn_d = din("norm_ffn_g", [D])
    w_gr_d = din("w_group_router", [D, 4])
    b_gr_d = din("b_group_router", [4])
    w_er_d = din("w_expert_router", [D, 16])
    b_er_d = din("b_expert_router", [16])
    w_eg_d = din("w_exp_gate", [NEXP, D, 512])
    w_eu_d = din("w_exp_up", [NEXP, D, 512])
    w_ed_d = din("w_exp_down", [NEXP, 512, D])
    g_fin_d = din("norm_final_g", [D])
    c_ident_d = din("c_ident", [128, 128])
    c_nti_d = din("c_nti", [128, 128])
    c_ntl_d = din("c_ntl", [128, 128])
    c_dmask_d = din("c_dmask", [128, 128])
    c_cos_d = din("c_cos", [128, 16, 32])
    c_sin_d = din("c_sin", [128, 16, 2, 32])
    c_decqk_d = din("c_decqk", [128, 16])
    c_mret_d = din("c_mret", [128, 8, 128])
    c_g128_d = din("c_g128", [128, 4])
    out_d = nc.dram_tensor("out", [SEQ_PER_CORE, T, D], F32, kind="ExternalOutput").ap()
    hscr_d = nc.dram_tensor("hscr", [SEQ_PER_CORE, T, D], F32).ap()
    dbg_d = nc.dram_tensor("dbg", [128, 16384], F32, kind="ExternalOutput").ap() if stop else None

    stack = ExitStack()
    with stack:
        def sb(name, nbytes):
            return stack.enter_context(nc.sbuf_tensor(name, [128, nbytes // 4], F32))

        R_act = sb("R_act", 32768)
        R_c = sb("R_c", 49152)
        R_m = sb("R_m", 32768)
        R_big = sb("R_big", 65536)
        R_misc = sb("R_misc", 28672)
        PSP = [stack.enter_context(nc.psum_tensor("psp%d" % i, [128, 1024], F32)) for i in range(4)]

        def bank(i):
            return PSP[i // 2][:, (i % 2) * 512:(i % 2) * 512 + 512]

        def bank_bf(i):
            return bank(i).bitcast(BF16)

        actT = _view(R_act, 0, BF16, 8, T)
        ysbT = _view(R_c, 0, BF16, 4, T)
        uT = _view(R_c, 16384, BF16, 8, T)
        mT = _view(R_m, 0, BF16, 8, T)

        misc = Carver(R_misc, 28672)
        xt = [misc.get(F32, D) for _ in range(2)]
        xn_bf = [misc.get(BF16, D) for _ in range(2)]
        junk = misc.get(BF16, D)
        gb = misc.get(F32, D)
        ident = misc.get(BF16, 128)
        nti = misc.get(BF16, 128)
        ntl = misc.get(BF16, 128)
        zero_bf = misc.get(BF16, 128)
        dmask = misc.get(F32, 128)
        decqk = misc.get(F32, 16)
        g128 = misc.get(F32, 4)
        bgate = misc.get(F32, 16)
        rngt = misc.get(F32, 8)
        eps_t = misc.get(F32, 1)
        wr_bf = misc.get(BF16, 8, 20)
        rbias = misc.get(F32, 20)
        ssq = [misc.get(F32, 2) for _ in range(4)]
        std = [misc.get(F32, 2) for _ in range(4)]
        rstd = [misc.get(F32, 2) for _ in range(4)]
        lg = misc.get(F32, 16, 20)
        r_gmax = misc.get(F32, 16)
        r_gd = misc.get(F32, 16, 4)
        r_gsum = misc.get(F32, 16)
        r_pg = misc.get(F32, 16)
        r_ohg = misc.get(F32, 16, 4)
        r_tmp = misc.get(F32, 16, 16)
        r_ig = misc.get(F32, 16, 4)
        r_ig2 = misc.get(F32, 16, 4)
        r_m1 = misc.get(F32, 16)
        r_m2 = misc.get(F32, 16)
        r_oh1 = misc.get(F32, 16, 4)
        r_oh2 = misc.get(F32, 16, 4)
        r_d = misc.get(F32, 16)
        r_w1 = misc.get(F32, 16)
        r_w2 = misc.get(F32, 16)
        r_cig = misc.get(F32, 16, 4)
        comb = misc.get(F32, 16, 16)

        def mm(out, lhsT, rhs, start=True, stop=True, skip=False):
            S.add("pe", lambda e: e.matmul(out, lhsT=lhsT, rhs=rhs, start=start, stop=stop, skip_group_check=skip),
                  reads=[lhsT, rhs], writes=[out])

        def tr(out, in_):
            S.add("pe", lambda e: e.transpose(out, in_, ident), reads=[in_, ident], writes=[out])

        def act(out, in_, func, scale=1.0, bias=None, accum_out=None):
            reads = [in_]
            writes = [out]
            kw = {}
            if bias is not None:
                kw["bias"] = bias
                if not isinstance(bias, (int, float)):
                    reads.append(bias)
            if not isinstance(scale, (int, float)):
                reads.append(scale)
            if accum_out is not None:
                kw["accum_out"] = accum_out
                writes.append(accum_out)
            S.add("act", lambda e: e.activation(out=out, in_=in_, func=func, scale=scale, **kw),
                  reads=reads, writes=writes)

        def tt(out, in0, in1, op, eng="dve"):
            S.add(eng, lambda e: e.tensor_tensor(out=out, in0=in0, in1=in1, op=op),
                  reads=[in0, in1], writes=[out])

        def ts(out, in0, s1, op0, s2=None, op1=None, eng="dve"):
            reads = [in0]
            if not isinstance(s1, (int, float)):
                reads.append(s1)
            if s2 is not None and not isinstance(s2, (int, float)):
                reads.append(s2)
            if op1 is None:
                S.add(eng, lambda e: e.tensor_scalar(out=out, in0=in0, scalar1=s1, scalar2=None, op0=op0),
                      reads=reads, writes=[out])
            else:
                S.add(eng, lambda e: e.tensor_scalar(out=out, in0=in0, scalar1=s1, scalar2=s2, op0=op0, op1=op1),
                      reads=reads, writes=[out])

        def stt(out, in0, scalar, in1, op0, op1, eng="dve"):
            reads = [in0, in1]
            if not isinstance(scalar, (int, float)):
                reads.append(scalar)
            S.add(eng, lambda e: e.scalar_tensor_tensor(out=out, in0=in0, scalar=scalar, in1=in1, op0=op0, op1=op1),
                  reads=reads, writes=[out])

        def cp(out, in_, eng="dve"):
            S.add(eng, lambda e: e.tensor_copy(out=out, in_=in_), reads=[in_], writes=[out])

        def red(out, in_, op, eng="dve"):
            S.add(eng, lambda e: e.tensor_reduce(out=out, in_=in_, axis=AX.X, op=op), reads=[in_], writes=[out])

        def recip(out, in_):
            S.add("dve", lambda e: e.reciprocal(out=out, in_=in_), reads=[in_], writes=[out])

        def memset(ap, val, eng="dve"):
            S.add(eng, lambda e: e.memset(ap, val), writes=[ap])

        def dma_in(eng, out, in_, slot, extra=()):
            return S.add(eng, lambda e: e.dma_start(out=out, in_=in_), writes=[out], slot=slot, extra_deps=extra)

        def dma_out(eng, out, in_, slot):
            return S.add(eng, lambda e: e.dma_start(out=out, in_=in_), reads=[in_], slot=slot)

        def wview(dram2d, r0, nrows, c0, ncols):
            return dram2d[r0:r0 + nrows, c0:c0 + ncols].rearrange("(kc p) n -> p kc n", p=128)

        memset(zero_bf, 0.0, eng="pool")
        memset(eps_t, EPS, eng="pool")
        dma_in("pool", ident, c_ident_d, "c0")
        dma_in("pool", nti, c_nti_d, "c1")
        dma_in("pool", ntl, c_ntl_d, "c2")
        def late_consts():
            dma_in("sp", dmask, c_dmask_d, "c3")
            dma_in("sp", decqk, c_decqk_d, "c4")
            dma_in("sp", g128, c_g128_d, "c5")
            dma_in("sp", bgate, bgate_d, "c6")
            dma_in("sp", rngt, rng_d, "c7")
            dma_in("sp", rbias[:, 0:4], b_gr_d.partition_broadcast(128), "c10")
            dma_in("sp", rbias[:, 4:20], b_er_d.partition_broadcast(128), "c11")

        dma_in("pool", wr_bf[:, :, 0:4], wview(w_gr_d, 0, D, 0, 4), "c8")
        dma_in("pool", wr_bf[:, :, 4:20], wview(w_er_d, 0, D, 0, 16), "c9")

        def rmsnorm_stats(src, k, inv_n, nparts=1):
            act(junk[:, 0:src.shape[1]], src, AF.Square, accum_out=ssq[k][:, 0:1])
            act(std[k][:, 0:1], ssq[k][:, 0:1], AF.Sqrt, scale=inv_n, bias=eps_t[:, 0:1])
            recip(rstd[k][:, 0:1], std[k][:, 0:1])

        def dump(view, n):
            S.add("pool", lambda e: e.dma_start(out=dbg_d[:, 0:n], in_=view), reads=[view], slot="dbg")

        def ck(tag, view, n):
            if stop == tag:
                dump(view, n)
                raise _Stop()

        def seq_body(b):
            big = Carver(R_big, 65536)

            dma_in("sp", gb, g_mix_d.partition_broadcast(128), "gb")
            xt4 = [xt[0], xt[1], _view(R_m, 0, F32, D), _view(R_m, 4096, F32, D)]

            def p1_a(i):
                s = i % 4
                dma_in("sp", xt4[s], x_d[b, i * 128:(i + 1) * 128, :], "xt%d" % s)
                rmsnorm_stats(xt4[s], s, 1.0 / D)
                stt(xn_bf[i % 2], xt4[s], rstd[s][:, 0:1], gb, ALU.mult, ALU.mult)

            def p1_b(i):
                pb = bank_bf(i % 2)
                for kc in range(KC):
                    tr(pb[:, kc * 128:(kc + 1) * 128], xn_bf[i % 2][:, kc * 128:(kc + 1) * 128])
                act(actT[:, :, i * 128:(i + 1) * 128], pb.rearrange("p (a b) -> p a b", a=8), AF.Copy)

            for i in range(NT + 1):
                if i < NT:
                    p1_a(i)
                if i >= 1:
                    p1_b(i - 1)

            if b == 0:
                late_consts()

            if stop == "P1":
                dump(_view(R_act, 0, BF16, 16384), 16384)
                return

            big.reset(0)
            sbw = [big.get(BF16, 8, 384) for _ in range(2)]
            qT_s = [big.get(BF16, T) for _ in range(2)]
            kT_s = [[big.get(BF16, T) for _ in range(2)] for _ in range(2)]
            vpad_s = [[big.get(BF16, 16, 128) for _ in range(2)] for _ in range(2)]
            NES = 4
            rm2 = Carver(R_m, 32768)
            rm2.reset(8192)
            e_s = [rm2.get(F32, 512) for _ in range(NES)]
            sp_s = [rm2.get(BF16, 512) for _ in range(NES)]
            xs_s = [rm2.get(F32, 512) for _ in range(2)]
            W_s = [rm2.get(BF16, 512) for _ in range(2)]

            def load_sbw(hp):
                sl = hp % 2
                for j in range(3):
                    dma_in("pool", sbw[sl][:, :, j * 128:(j + 1) * 128],
                           wview(w_in_d, 0, D, j * 512 + hp * 128, 128), "sbw%d_%d" % (sl, j))

            for ps_ in range(2):
                memset(vpad_s[ps_][0][:, :, 64:128], 0.0)
                memset(vpad_s[ps_][1][:, :, 0:64], 0.0)
                memset(kT_s[ps_][0][64:128, :], 0.0)
                memset(kT_s[ps_][1][0:64, :], 0.0)

            def proj_items(hp):
                w = sbw[hp % 2]
                qT, kT, vpad = qT_s[hp % 2], kT_s[hp % 2], vpad_s[hp % 2]
                items = []
                cnt = [0]
                for which, dst, scl in ((0, qT, 0.125), (1, kT, 1.0)):
                    for tc in range(4):
                        def it(which=which, dst=dst, scl=scl, tc=tc):
                            pbk = bank(cnt[0] % 2)
                            cnt[0] += 1
                            for kc in range(KC):
                                mm(pbk, w[:, kc, which * 128:(which + 1) * 128], actT[:, kc, tc * 512:(tc + 1) * 512],
                                   start=(kc == 0), stop=(kc == KC - 1))
                            if which == 0:
                                ts(dst[:, tc * 512:(tc + 1) * 512], pbk, scl, ALU.mult)
                            else:
                                ts(dst[0][0:64, tc * 512:(tc + 1) * 512], pbk[0:64, :], scl, ALU.mult)
                                ts(dst[1][64:128, tc * 512:(tc + 1) * 512], pbk[64:128, :], scl, ALU.mult)
                        items.append(it)
                for quad in range(4):
                    def it(quad=quad):
                        pbk = bank(cnt[0] % 2)
                        cnt[0] += 1
                        for j in range(4):
                            i = quad * 4 + j
                            for kc in range(KC):
                                mm(pbk[:, j * 128:(j + 1) * 128], actT[:, kc, i * 128:(i + 1) * 128], w[:, kc, 256:384],
                                   start=(kc == 0), stop=(kc == KC - 1))
                        pv = pbk.rearrange("p (a b) -> p a b", a=4)
                        cp(vpad[0][:, quad * 4:(quad + 1) * 4, 0:64], pv[:, :, 0:64])
                        cp(vpad[1][:, quad * 4:(quad + 1) * 4, 64:128], pv[:, :, 64:128])
                    items.append(it)
                return items

            load_sbw(0)
            load_sbw(1)
            for it_ in proj_items(0):
                it_()
            for hp in range(4):
                qT, kT, vpad = qT_s[hp % 2], kT_s[hp % 2], vpad_s[hp % 2]
                nxt = proj_items(hp + 1) if hp + 1 < 4 else []
                units = [(c, kb, hl) for c in range(4) for kb in range(4 * c + 3, -1, -1) for hl in range(2)]
                NU = len(units)

                def geom(u):
                    c, kb, hl = units[u]
                    lo = max(0, kb - 4 * c) * 128
                    return c, kb, hl, lo, 512 - lo

                def s_qk(u):
                    c, kb, hl, lo, n = geom(u)
                    zb = bank(2 + u % 2)
                    mm(zb[:, 0:n], kT[hl][:, kb * 128:(kb + 1) * 128], qT[:, c * 512 + lo:(c + 1) * 512])

                def s_act1(u):
                    c, kb, hl, lo, n = geom(u)
                    zb = bank(2 + u % 2)
                    ee = e_s[u % NES]
                    spp = sp_s[u % NES]
                    act(ee[:, 0:n], zb[:, 0:n], AF.Exp)
                    act(spp[:, 0:n], ee[:, 0:n], AF.Ln, bias=1.0)
                    if kb >= 4 * c:
                        tt(ee[:, 0:128], ee[:, 0:128], dmask, ALU.mult)
                        tt(spp[:, 0:128], spp[:, 0:128], dmask, ALU.mult)

                def s_nti(u):
                    c, kb, hl, lo, n = geom(u)
                    Sb = bank(4 + hl)
                    if kb == 4 * c + 3:
                        mm(Sb, zero_bf, qT[:, 0:512], start=True, stop=True)
                    mm(Sb[:, lo:512], nti, sp_s[u % NES][:, 0:n], start=False, stop=True, skip=True)

                def s_exps(u):
                    c, kb, hl, lo, n = geom(u)
                    act(xs_s[u % 2][:, 0:n], bank(4 + hl)[:, lo:512], AF.Exp)

                def warm(k):
                    for _ in range(k):
                        mm(bank(7), zero_bf, qT[:, 0:512], start=True, stop=True)

                def s_ntl(u):
                    c, kb, hl, lo, n = geom(u)
                    warm(NWARM)
                    mm(bank(4 + hl)[:, lo:512], ntl, sp_s[u % NES][:, 0:n], start=False, stop=True, skip=True)

                def s_w(u):
                    c, kb, hl, lo, n = geom(u)
                    tt(W_s[u % 2][:, 0:n], e_s[u % NES][:, 0:n], xs_s[u % 2][:, 0:n], ALU.mult)

                def s_pv(u):
                    c, kb, hl, lo, n = geom(u)
                    Yb = bank(6 + c % 2)
                    if kb == 4 * c + 3 and hl == 0:
                        mm(Yb, zero_bf, qT[:, 0:512], start=True, stop=False)
                    last = (kb == 0 and hl == 1)
                    mm(Yb[:, lo:512], vpad[hl][:, kb, :], W_s[u % 2][:, 0:n], start=False, stop=last)
                    if last:
                        act(ysbT[:, hp, c * 512:(c + 1) * 512], Yb, AF.Copy)

                s_qk(0)
                for i in range(NU + 2):
                    if 0 <= i - 1 < NU:
                        s_nti(i - 1)
                    if i + 1 < NU:
                        s_qk(i + 1)
                    if i < NU:
                        s_act1(i)
                    if 0 <= i - 1 < NU:
                        s_exps(i - 1)
                    if 0 <= i - 2 < NU:
                        s_ntl(i - 2)
                        s_w(i - 2)
                        s_pv(i - 2)
                    if nxt and i >= 4 and (i - 4) % 6 == 0 and (i - 4) // 6 < len(nxt):
                        nxt[(i - 4) // 6]()
                    if i == 12 and hp + 2 < 4:
                        load_sbw(hp + 2)

            if stop == "P2":
                dump(_view(R_c, 0, BF16, 8192), 8192)
                return

            big.reset(0)
            retw = [big.get(BF16, 8, 768) for _ in range(2)]
            cosT = big.get(F32, 16, 32)
            sinS = big.get(F32, 16, 64)
            mret = big.get(F32, 8, 128)
            NS1 = 4
            t1_s = [big.get(F32, 256) for _ in range(NS1)]
            t2_s = [big.get(F32, 256) for _ in range(NS1)]
            qkd_s = [big.get(BF16, 256) for _ in range(NS1)]
            v_r_s = [big.get(BF16, 256) for _ in range(NS1)]
            sg_s = [big.get(F32, 256) for _ in range(5)]
            qkT_s = [big.get(BF16, 256) for _ in range(NS1)]
            Pm_s = [big.get(BF16, 256) for _ in range(2)]
            u_s = [big.get(BF16, 256) for _ in range(2)]
            Ysb_s = [big.get(F32, 256) for _ in range(4)]
            state_f = big.get(F32, 256)
            state_bd = big.get(BF16, 256)
            mhalf = big.get(F32, 2)
            var_s = [big.get(F32, 2) for _ in range(4)]

            def load_retw(hp):
                sl = hp % 2
                dma_in("pool", retw[sl][:, :, 0:128], wview(w_in_d, 0, D, 1536 + hp * 128, 128), "retw%d_0" % sl)
                dma_in("pool", retw[sl][:, :, 128:256], wview(w_in_d, 0, D, 2048 + hp * 128, 128), "retw%d_1" % sl)
                dma_in("pool", retw[sl][:, :, 256:512], wview(w_in_d, 0, D, 2560 + hp * 256, 256), "retw%d_2" % sl)
                dma_in("pool", retw[sl][:, :, 512:768], wview(w_in_d, 0, D, 3584 + hp * 256, 256), "retw%d_3" % sl)

            dma_in("sp", cosT, c_cos_d, "cos")
            dma_in("sp", sinS, c_sin_d.rearrange("p a b c -> p a (b c)"), "sin")
            dma_in("sp", mret, c_mret_d, "mret")
            memset(mhalf, -0.5, eng="pool")
            load_retw(0)
            if True:
                def st1(g, part):
                    hp, n = divmod(g, NT)
                    w = retw[hp % 2]
                    if part == 0 and n == 0 and hp + 1 < 4:
                        load_retw(hp + 1)
                    k = g % NS1
                    t1, t2, qkd, v_r, sg, qkT = t1_s[k], t2_s[k], qkd_s[k], v_r_s[k], sg_s[g % 5], qkT_s[k]
                    A = bank(0)
                    B = bank(1)
                    if part == 0:
                        for kc in range(KC):
                            mm(A, actT[:, kc, n * 128:(n + 1) * 128], w[:, kc, 0:512], start=(kc == 0), stop=(kc == KC - 1))
                        for kc in range(KC):
                            mm(B[:, 0:256], actT[:, kc, n * 128:(n + 1) * 128], w[:, kc, 512:768],
                               start=(kc == 0), stop=(kc == KC - 1))
                        return
                    if part == 2:
                        Tb = bank_bf(2)
                        tr(Tb[:, 0:128], qkd[:, 0:128])
                        tr(Tb[:, 128:256], qkd[:, 128:256])
                        act(qkT, Tb[:, 0:256], AF.Copy)
                        return
                    A4 = A[:, 0:256].rearrange("p (g h i) -> p g h i", g=4, h=2, i=32)
                    t1_4 = t1.rearrange("p (g h i) -> p g h i", g=4, h=2, i=32)
                    t2_4 = t2.rearrange("p (g h i) -> p g h i", g=4, h=2, i=32)
                    cos_b = cosT[:, n, :].unsqueeze(1).unsqueeze(1).broadcast_to([128, 4, 2, 32])
                    tt(t1_4, A4, cos_b, ALU.mult)
                    for hf in range(2):
                        sin_b = sinS[:, n, hf * 32:(hf + 1) * 32].unsqueeze(1).broadcast_to([128, 4, 32])
                        tt(t2_4[:, :, hf, :], A4[:, :, 1 - hf, :], sin_b, ALU.mult)
                    cp(v_r, A[:, 256:512])
                    act(sg, B[:, 0:256], AF.Silu)
                    tt(t1, t1, t2, ALU.add)
                    dq = decqk[:, hp * 4:(hp + 1) * 4].unsqueeze(2).broadcast_to([128, 4, 64])
                    tt(qkd.rearrange("p (g j) -> p g j", g=4), t1.rearrange("p (g j) -> p g j", g=4), dq, ALU.mult)

                def st2(g):
                    hp, n = divmod(g, NT)
                    if n == 0:
                        memset(state_f, 0.0)
                        memset(state_bd, 0.0)
                    k = g % NS1
                    k2 = g % 2
                    qkd, v_r, qkT, Pm = qkd_s[k], v_r_s[k], qkT_s[k], Pm_s[k2]
                    qdT = qkT[:, 0:128]
                    kdT = qkT[:, 128:256]
                    Scs = [bank(3), bank(7)]
                    for hl in range(2):
                        r0 = hl * 64
                        mm(Scs[hl][:, 0:128], kdT[r0:r0 + 64, :], qdT[r0:r0 + 64, :])
                    KV = bank(5)
                    mm(KV[:, 0:256], qkd[:, 128:256], v_r)
                    Y = bank(4)
                    mm(Y[:, 0:256], qdT, state_bd, start=True, stop=False)
                    for hl in range(2):
                        tt(Pm[:, hl * 128:(hl + 1) * 128], Scs[hl][:, 0:128], mret[:, 2 * hp + hl, :], ALU.mult)
                    for hl in range(2):
                        mm(Y[:, hl * 128:(hl + 1) * 128], Pm[:, hl * 128:(hl + 1) * 128], v_r[:, hl * 128:(hl + 1) * 128],
                           start=False, stop=(hl == 1))
                    for hl in range(2):
                        pr = slice(hl * 64, hl * 64 + 64)
                        cr = slice(hl * 128, hl * 128 + 128)
                        stt(state_f[pr, cr], state_f[pr, cr], g128[pr, hp:hp + 1], KV[pr, cr], ALU.mult, ALU.add)
                        act(state_bd[pr, cr], state_f[pr, cr], AF.Copy)
                    k4 = g % 4
                    for hl in range(2):
                        act(junk[:, 0:128], Y[:, hl * 128:(hl + 1) * 128], AF.Square, accum_out=ssq[k4][:, hl:hl + 1])
                    act(Ysb_s[k4], Y[:, 0:256], AF.Copy)

                def st2b(g):
                    k4 = g % 4
                    ts(var_s[k4], ssq[k4], 1.0 / 128, ALU.mult, EPS, ALU.add)
                    tt(rstd[k4], var_s[k4], mhalf, ALU.pow, eng="pool")

                def st3(g):
                    hp, n = divmod(g, NT)
                    k = g % NS1
                    k2 = g % 2
                    sg, u_t = sg_s[g % 5], u_s[k2]
                    k4 = g % 4
                    Ys = Ysb_s[k4]
                    for hl in range(2):
                        cr = slice(hl * 128, hl * 128 + 128)
                        stt(u_t[:, cr], Ys[:, cr], rstd[k4][:, hl:hl + 1], sg[:, cr], ALU.mult, ALU.mult)
                    T2 = bank_bf(6)
                    for hl in range(2):
                        tr(T2[:, hl * 128:(hl + 1) * 128], u_t[:, hl * 128:(hl + 1) * 128])
                    for hl in range(2):
                        h = 2 * hp + hl
                        act(uT[:, h, n * 128:(n + 1) * 128], T2[:, hl * 128:(hl + 1) * 128], AF.Copy, scale=rngt[:, h:h + 1])

                NG = 4 * NT
                for it_n in range(NG + 4):
                    if 0 <= it_n - 3 < NG:
                        st2b(it_n - 3)
                    if 0 <= it_n - 4 < NG:
                        st3(it_n - 4)
                    if it_n < NG:
                        st1(it_n, 0)
                        st1(it_n, 1)
                    if 0 <= it_n - 1 < NG:
                        st2(it_n - 1)
                    if it_n < NG:
                        st1(it_n, 2)

            if stop == "P3":
                dump(_view(R_c, 16384, BF16, 16384), 16384)
                return

            big.reset(0)
            a4w = []
            for _ in range(2):
                a4w.append(dict(wso=big.get(BF16, 4, 128), wro=big.get(BF16, 8, 128),
                                wg1=big.get(BF16, 8, 128), wg2=big.get(BF16, 8, 128)))
            a4s = []
            for _ in range(2):
                a4s.append(dict(s1=big.get(F32, 512), s2=big.get(F32, 512), m1=big.get(F32, 512), t=big.get(F32, 512)))
            wout = big.get(BF16, 8, D)

            def load_a4w(fc):
                sl = fc % 2
                dma_in("pool", a4w[sl]["wso"], wview(w_sbo_d, 0, 512, fc * 128, 128), "a4w%d_0" % sl)
                dma_in("pool", a4w[sl]["wro"], wview(w_ro_d, 0, D, fc * 128, 128), "a4w%d_1" % sl)
                dma_in("pool", a4w[sl]["wg1"], wview(w_gate_d, 0, D, fc * 128, 128), "a4w%d_2" % sl)
                dma_in("pool", a4w[sl]["wg2"], wview(w_gate_d, 0, D, 1024 + fc * 128, 128), "a4w%d_3" % sl)

            load_a4w(0)
            dma_in("pool", wout, wview(w_out_d, 0, D, 0, D), "wout")
            it = 0
            for fc in range(8):
                if fc + 1 < 8:
                    load_a4w(fc + 1)
                w = a4w[fc % 2]
                for tc in range(4):
                    cols = slice(tc * 512, (tc + 1) * 512)
                    bb = 4 * (it % 2)
                    sc = a4s[it % 2]
                    it += 1
                    A, B, C, Dd = bank(bb), bank(bb + 1), bank(bb + 2), bank(bb + 3)
                    for kc in range(4):
                        mm(A, w["wso"][:, kc, :], ysbT[:, kc, cols], start=(kc == 0), stop=(kc == 3))
                    for kc in range(8):
                        mm(B, w["wro"][:, kc, :], uT[:, kc, cols], start=(kc == 0), stop=(kc == 7))
                    for kc in range(8):
                        mm(C, w["wg1"][:, kc, :], actT[:, kc, cols], start=(kc == 0), stop=(kc == 7))
                    for kc in range(8):
                        mm(Dd, w["wg2"][:, kc, :], actT[:, kc, cols], start=(kc == 0), stop=(kc == 7))
                    act(sc["s1"], C, AF.Sigmoid, bias=bgate[:, fc:fc + 1])
                    act(sc["s2"], Dd, AF.Sigmoid, bias=bgate[:, 8 + fc:9 + fc])
                    tt(sc["m1"], A, sc["s1"], ALU.mult)
                    tt(sc["t"], B, sc["s2"], ALU.mult)
                    tt(mT[:, fc, cols], sc["m1"], sc["t"], ALU.add)

            if stop == "P4":
                dump(_view(R_m, 0, BF16, 16384), 16384)
                return

            moew = []
            for sl in range(2):
                base = sl * 24576
                moew.append(dict(wg=_view(R_c, base, BF16, 8, 512), wu=_view(R_c, base + 8192, BF16, 8, 512),
                                 wd=_view(R_c, base + 16384, BF16, 4, D)))
            def load_moew(e):
                sl = e % 2
                dma_in("pool", moew[sl]["wg"], w_eg_d[e].rearrange("(kc p) n -> p kc n", p=128), "moew%d_0" % sl)
                dma_in("pool", moew[sl]["wu"], w_eu_d[e].rearrange("(kc p) n -> p kc n", p=128), "moew%d_1" % sl)
                dma_in("pool", moew[sl]["wd"], w_ed_d[e].rearrange("(kc p) n -> p kc n", p=128), "moew%d_2" % sl)

            load_moew(0)

            dma_in("sp", gb, g_ffn_d.partition_broadcast(128), "gb")
            xt5 = [xt[0], xt[1], big.get(F32, D), big.get(F32, D)]

            h_store = {}

            def p5_a(i):
                s = i % 4
                dma_in("sp", xt5[s], x_d[b, i * 128:(i + 1) * 128, :], "xt%d" % s)
                pp = PSP[i % 2]
                for half in range(2):
                    for kc in range(KC):
                        mm(pp[:, half * 512:(half + 1) * 512], mT[:, kc, i * 128:(i + 1) * 128],
                           wout[:, kc, half * 512:(half + 1) * 512], start=(kc == 0), stop=(kc == KC - 1))
                tt(xt5[s], xt5[s], pp[:, :], ALU.add)
                h_store[i] = dma_out("pool", hscr_d[b, i * 128:(i + 1) * 128, :], xt5[s], "xs%d" % s)
                rmsnorm_stats(xt5[s], s, 1.0 / D)
                stt(xn_bf[i % 2], xt5[s], rstd[s][:, 0:1], gb, ALU.mult, ALU.mult)

            def p5_b(i):
                pb = bank_bf(4 + i % 2)
                for kc in range(KC):
                    tr(pb[:, kc * 128:(kc + 1) * 128], xn_bf[i % 2][:, kc * 128:(kc + 1) * 128])
                act(actT[:, :, i * 128:(i + 1) * 128], pb.rearrange("p (a b) -> p a b", a=8), AF.Copy)

            for i in range(NT + 1):
                if i < NT:
                    p5_a(i)
                if i >= 1:
                    p5_b(i - 1)

            if stop == "P5":
                dump(_view(R_act, 0, BF16, 16384), 16384)
                return

            yacc = _view(R_big, 0, F32, NT, D)
            mcar = Carver(R_m, 32768)
            hT = [mcar.get(BF16, 4, 512) for _ in range(2)]
            sgm = [mcar.get(F32, 512) for _ in range(2)]

            RB = bank(0)
            for i in range(NT):
                for kc in range(KC):
                    mm(RB[:, i * 20:(i + 1) * 20], actT[:, kc, i * 128:(i + 1) * 128], wr_bf[:, kc, :],
                       start=(kc == 0), stop=(kc == KC - 1))
            tt(lg, RB[:, 0:320].rearrange("p (a b) -> p a b", a=16), rbias.unsqueeze(1).broadcast_to([128, 16, 20]), ALU.add)
            GL = lg[:, :, 0:4]
            EL = lg[:, :, 4:20].rearrange("p t (g e) -> p t g e", g=4)

            def b4(v):
                return v.unsqueeze(2).broadcast_to([128, 16, 4])

            red(r_gmax, GL, ALU.max)
            tt(r_gd, GL, b4(r_gmax), ALU.subtract)
            act(r_gd, r_gd, AF.Exp)
            red(r_gsum, r_gd, ALU.add)
            recip(r_pg, r_gsum)
            tt(r_ohg, GL, b4(r_gmax), ALU.is_equal)
            tmp4 = r_tmp.rearrange("p t (g e) -> p t g e", g=4)
            tt(tmp4, EL, r_ohg.unsqueeze(3).broadcast_to([128, 16, 4, 4]), ALU.mult)
            red(r_ig, r_tmp.rearrange("p t (g e) -> p t e g", g=4), ALU.add)
            red(r_m1, r_ig, ALU.max)
            tt(r_oh1, r_ig, b4(r_m1), ALU.is_equal)
            stt(r_ig2, r_oh1, -1.0e30, r_ig, ALU.mult, ALU.add)
            red(r_m2, r_ig2, ALU.max)
            tt(r_oh2, r_ig2, b4(r_m2), ALU.is_equal)
            tt(r_d, r_m2, r_m1, ALU.subtract)
            act(r_d, r_d, AF.Exp)
            ts(r_w1, r_d, 1.0, ALU.add)
            recip(r_w1, r_w1)
            tt(r_w2, r_d, r_w1, ALU.mult)
            tt(r_w1, r_w1, r_pg, ALU.mult)
            tt(r_w2, r_w2, r_pg, ALU.mult)
            tt(r_cig, r_oh1, b4(r_w1), ALU.mult)
            tt(r_oh2, r_oh2, b4(r_w2), ALU.mult)
            tt(r_cig, r_cig, r_oh2, ALU.add)
            tt(comb.rearrange("p t (g e) -> p t g e", g=4), r_ohg.unsqueeze(3).broadcast_to([128, 16, 4, 4]),
               r_cig.unsqueeze(2).broadcast_to([128, 16, 4, 4]), ALU.mult)

            gcount = 0
            for e in range(NEXP):
                if e + 1 < NEXP:
                    load_moew(e + 1)
                w = moew[e % 2]
                for tc in range(4):
                    cols = slice(tc * 512, (tc + 1) * 512)
                    hh = hT[tc % 2]
                    for fcb in range(4):
                        G = bank(gcount % 2)
                        U = bank(2 + gcount % 2)
                        sgt = sgm[gcount % 2]
                        gcount += 1
                        for kc in range(KC):
                            mm(G, w["wg"][:, kc, fcb * 128:(fcb + 1) * 128], actT[:, kc, cols],
                               start=(kc == 0), stop=(kc == KC - 1))
                        for kc in range(KC):
                            mm(U, w["wu"][:, kc, fcb * 128:(fcb + 1) * 128], actT[:, kc, cols],
                               start=(kc == 0), stop=(kc == KC - 1))
                        act(sgt, G, AF.Silu)
                        tt(hh[:, fcb, :], sgt, U, ALU.mult)
                    for j in range(4):
                        i = tc * 4 + j
                        yp = PSP[2 + i % 2]
                        for half in range(2):
                            for fcb in range(4):
                                mm(yp[:, half * 512:(half + 1) * 512], hh[:, fcb, j * 128:(j + 1) * 128],
                                   w["wd"][:, fcb, half * 512:(half + 1) * 512], start=(fcb == 0), stop=(fcb == 3))
                        if e == 0:
                            ts(yacc[:, i, :], yp[:, :], comb[:, i, e:e + 1], ALU.mult)
                        else:
                            stt(yacc[:, i, :], yp[:, :], comb[:, i, e:e + 1], yacc[:, i, :], ALU.mult, ALU.add)

            dma_in("sp", gb, g_fin_d.partition_broadcast(128), "gb")
            xt6 = [xt[0], xt[1], _view(R_m, 16384, F32, D), _view(R_m, 20480, F32, D)]

            def fin_load(i):
                dma_in("sp", xt6[i % 4], hscr_d[b, i * 128:(i + 1) * 128, :], "xt%d" % (i % 4), extra=[h_store[i]])

            for i in range(3):
                fin_load(i)
            for i in range(NT):
                s = i % 4
                tt(xt6[s], xt6[s], yacc[:, i, :], ALU.add)
                rmsnorm_stats(xt6[s], s, 1.0 / D)
                stt(xt6[s], xt6[s], rstd[s][:, 0:1], gb, ALU.mult, ALU.mult)
                if i + 3 < NT:
                    fin_load(i + 3)
                dma_out("pool", out_d[b, i * 128:(i + 1) * 128, :], xt6[s], "xs%d" % s)

        try:
            for b in range(nseq):
                seq_body(b)
        except _Stop:
            pass

        S.finish()
        S.emit(nc, stack)
    return nc


def _constants():
    c = {}
    c["c_ident"] = np.eye(128, dtype=np.float32)
    j = np.arange(128)[:, None]
    s = np.arange(128)[None, :]
    c["c_nti"] = np.where(j >= s, -1.0, 0.0).astype(np.float32)
    c["c_ntl"] = np.where(j < s, -1.0, 0.0).astype(np.float32)
    c["c_dmask"] = np.where(j < s, 1.0, 0.0).astype(np.float32)
    inv_freq = (np.float32(10000.0) ** (-(np.arange(32, dtype=np.float32) / np.float32(32)))).astype(np.float32)
    pos = np.arange(T, dtype=np.float32)
    ang = (pos[:, None] * inv_freq[None, :]).astype(np.float32).astype(np.float64)
    cos = np.cos(ang).reshape(16, 128, 32).transpose(1, 0, 2)
    sin = np.sin(ang).reshape(16, 128, 32).transpose(1, 0, 2)
    c["c_cos"] = np.ascontiguousarray(cos).astype(np.float32)
    c["c_sin"] = np.ascontiguousarray(np.stack([-sin, sin], axis=2)).astype(np.float32)
    log_gamma = np.log(1.0 - 2.0 ** (-5.0 - np.arange(8, dtype=np.float64)))
    p = np.arange(128, dtype=np.float64)
    decqk = np.zeros((128, 16), np.float64)
    for hp in range(4):
        for hl in range(2):
            h = 2 * hp + hl
            decqk[:, hp * 4 + hl] = np.exp(log_gamma[h] * (p + 1.0))
            decqk[:, hp * 4 + 2 + hl] = 0.125 * np.exp(log_gamma[h] * (127.0 - p))
    c["c_decqk"] = decqk.astype(np.float32)
    m = np.arange(128)[:, None].astype(np.float64)
    cc = np.arange(128)[None, :].astype(np.float64)
    valid = (np.arange(128)[:, None] // 64) <= (np.arange(128)[None, :] // 64)
    mret = np.zeros((128, 8, 128), np.float64)
    for h in range(8):
        mret[:, h, :] = np.where(valid, np.exp(log_gamma[h] * (np.abs(cc - m) - (cc - m) - 128.0)), 0.0)
    c["c_mret"] = mret.astype(np.float32)
    g128 = np.zeros((128, 4), np.float64)
    for hp in range(4):
        g128[0:64, hp] = np.exp(log_gamma[2 * hp] * 128.0)
        g128[64:128, hp] = np.exp(log_gamma[2 * hp + 1] * 128.0)
    c["c_g128"] = g128.astype(np.float32)
    return c


_NC_CACHE = {}


def kernel(x, norm_mix_g, w_in, w_gate, b_gate, w_sb_out, w_ret_out, ret_norm_g, w_out,
           norm_ffn_g, w_group_router, b_group_router, w_expert_router, b_expert_router,
           w_exp_gate, w_exp_up, w_exp_down, norm_final_g):
    f = lambda a: np.ascontiguousarray(np.asarray(a, dtype=np.float32))
    if "nc" not in _NC_CACHE:
        _NC_CACHE["nc"] = build_program()
    nc = _NC_CACHE["nc"]
    shared = {
        "norm_mix_g": f(norm_mix_g).reshape(D),
        "w_in": f(w_in).reshape(D, 4608),
        "w_gate": f(w_gate).reshape(D, 2048),
        "b_gate_l": f(f(b_gate).reshape(16, 128).T),
        "w_sb_out": f(w_sb_out).reshape(512, D),
        "w_ret_out": f(w_ret_out).reshape(D, D),
        "ret_norm_g_l": f(f(ret_norm_g).reshape(8, 128).T),
        "w_out": f(w_out).reshape(D, D),
        "norm_ffn_g": f(norm_ffn_g).reshape(D),
        "w_group_router": f(w_group_router).reshape(D, 4),
        "b_group_router": f(b_group_router).reshape(4),
        "w_expert_router": f(w_expert_router).reshape(D, 16),
        "b_expert_router": f(b_expert_router).reshape(16),
        "w_exp_gate": f(w_exp_gate).reshape(NEXP, D, 512),
        "w_exp_up": f(w_exp_up).reshape(NEXP, D, 512),
        "w_exp_down": f(w_exp_down).reshape(NEXP, 512, D),
        "norm_final_g": f(norm_final_g).reshape(D),
    }
    shared.update(_constants())
    xf = f(x)
    in_maps = []
    for c in range(NCORES):
        m = dict(shared)
        m["x"] = np.ascontiguousarray(xf[c * SEQ_PER_CORE:(c + 1) * SEQ_PER_CORE])
        in_maps.append(m)
    res = run_bass_kernel_spmd(nc, in_maps, core_ids=list(range(NCORES)))
    out = np.concatenate([np.asarray(r["out"], dtype=np.float32) for r in res.results], axis=0)
    return out.reshape(16, T, D)
```
